# Optimizing a Trainium2 kernel written in Bass

```python
import math
import jax, jax.numpy as jnp
from jax import lax
import numpy as np

D_MODEL = 2048
BATCH = 8
SEQ = 2048
DEPTH = 2

CTX_LEN = 256
GRID_W = 64
EPS = 1e-6

D_MIX = D_MODEL
CONV_W = D_MIX // 4
MLA_HEADS = 8
MLA_V_DIM = 128
MLA_W = MLA_HEADS * MLA_V_DIM
HYENA_W = D_MIX - CONV_W - MLA_W

CONV_KSIZE = 31

MLA_Q_RANK = 768
MLA_KV_RANK = 512
MLA_NOPE = 128
MLA_ROPE = 64
ROPE_FREQS = MLA_ROPE // 4
ROPE_THETA = 10000.0
MLA_SCALE = (MLA_NOPE + MLA_ROPE) ** -0.5
Q_BLOCK = 128

HY_ORDER = 2
HY_SHORT = 3
HY_EMB = 33
HY_BANDS = (HY_EMB - 1) // 2
HY_FFN = 64
HY_TARGET = 1e-2
HY_FAST_PCT = 0.3
HY_SLOW_PCT = 1.5
HY_MIN_DECAY = math.log(HY_TARGET) / HY_SLOW_PCT
HY_MAX_DECAY = math.log(HY_TARGET) / HY_FAST_PCT

N_EXPERTS = 16
N_GROUPS = 4
EXPERTS_PER_GROUP = N_EXPERTS // N_GROUPS
TOP_K = 2
D_FF_EXPERT = 1024

OFF_Q = 2 * CONV_W
OFF_KV = OFF_Q + MLA_Q_RANK
OFF_HY = OFF_KV + MLA_KV_RANK + MLA_ROPE
N_IN = OFF_HY + (HY_ORDER + 1) * HYENA_W

kernel_name = "hybrid_conv_mla_hyena_moe_dit"


def rmsnorm(x, g):
    xf = x.astype(jnp.float32)
    y = xf * lax.rsqrt(jnp.mean(xf * xf, axis=-1, keepdims=True) + EPS)
    return (y * g.astype(jnp.float32)).astype(x.dtype)


def layernorm(x, g, b):
    xf = x.astype(jnp.float32)
    mu = jnp.mean(xf, axis=-1, keepdims=True)
    var = jnp.mean(jnp.square(xf - mu), axis=-1, keepdims=True)
    y = (xf - mu) * lax.rsqrt(var + EPS)
    return (y * g.astype(jnp.float32) + b.astype(jnp.float32)).astype(x.dtype)


def modulation(cvec, w_ada, b_ada):
    m = jax.nn.silu(cvec) @ w_ada + b_ada
    return jnp.split(m[:, None, :], 6, axis=-1)


def modulate(h, shift, scale):
    return h * (1 + scale) + shift


def depthwise_conv(z, w, b):
    k = w.shape[0]
    y = lax.conv_general_dilated(z, w[:, None, :].astype(z.dtype), window_strides=(1,),
                                 padding=[(k // 2, k // 2)],
                                 dimension_numbers=('NWC', 'WIO', 'NWC'),
                                 feature_group_count=z.shape[-1])
    return y + b


def axial_rope_tables(L, dtype):
    rows = L // GRID_W
    r = jnp.repeat(jnp.arange(rows), GRID_W)
    col = jnp.tile(jnp.arange(GRID_W), rows)
    pos = jnp.stack([r, col], axis=-1).astype(jnp.float32)
    inv = ROPE_THETA ** (-jnp.arange(ROPE_FREQS, dtype=jnp.float32) / ROPE_FREQS)
    ang = pos[:, :, None] * inv
    return jnp.cos(ang).astype(dtype), jnp.sin(ang).astype(dtype)


def apply_rope(x, cos, sin):
    xs = x.reshape(x.shape[:-1] + (2, 2, ROPE_FREQS))
    x1, x2 = xs[..., 0, :], xs[..., 1, :]
    out = jnp.stack([x1 * cos - x2 * sin, x1 * sin + x2 * cos], axis=-2)
    return out.reshape(x.shape)


def conformer_conv(p, dw_w, dw_b, ln_g, ln_b):
    a, g = jnp.split(p, 2, axis=-1)
    z = depthwise_conv(a * jax.nn.sigmoid(g), dw_w, dw_b)
    return jax.nn.silu(layernorm(z, ln_g, ln_b))


def mla_q(p_q, g, w_uq, rope):
    B, L, _ = p_q.shape
    q = (rmsnorm(p_q, g) @ w_uq).reshape(B, L, MLA_HEADS, MLA_NOPE + MLA_ROPE)
    if rope is None:
        return q
    cos, sin = rope
    return jnp.concatenate([q[..., :MLA_NOPE],
                            apply_rope(q[..., MLA_NOPE:], cos[:, None], sin[:, None])], axis=-1)


def mla_kv(p_kv, g, w_ukv, rope):
    B, L, _ = p_kv.shape
    kv = (rmsnorm(p_kv[..., :MLA_KV_RANK], g) @ w_ukv).reshape(B, L, MLA_HEADS, MLA_NOPE + MLA_V_DIM)
    k_rope = p_kv[..., MLA_KV_RANK:]
    if rope is not None:
        k_rope = apply_rope(k_rope, *rope)
    k = jnp.concatenate([kv[..., :MLA_NOPE],
                         jnp.broadcast_to(k_rope[:, :, None, :], (B, L, MLA_HEADS, MLA_ROPE))], axis=-1)
    return k, kv[..., MLA_NOPE:]


def block_attention(q, k, v):
    B, L, H, Dq = q.shape
    nb = L // Q_BLOCK
    qb = q.reshape(B, nb, Q_BLOCK, H, Dq).transpose(1, 0, 2, 3, 4)

    def one_block(qi):
        s = jnp.einsum('bqhd,bkhd->bhqk', qi, k, preferred_element_type=jnp.float32) * MLA_SCALE
        prob = jax.nn.softmax(s, axis=-1).astype(v.dtype)
        return jnp.einsum('bhqk,bkhd->bqhd', prob, v)

    o = lax.map(one_block, qb)
    return o.transpose(1, 0, 2, 3, 4).reshape(B, L, H * v.shape[-1])


def hyena_filters(L, w1, b1, fr1, w2, b2, fr2, w3, b3):
    f32 = jnp.float32
    t = jnp.arange(L, dtype=f32)
    t_norm = t / max(L - 1, 1)
    ang = (2.0 * math.pi / L) * t[:, None] * jnp.linspace(1e-4, HY_BANDS - 1, HY_BANDS, dtype=f32)[None, :]
    z = jnp.concatenate([t_norm[:, None], jnp.cos(ang), -jnp.sin(ang)], axis=-1)
    h = jnp.sin(fr1.astype(f32) * (z @ w1.astype(f32) + b1.astype(f32)))
    h = jnp.sin(fr2.astype(f32) * (h @ w2.astype(f32) + b2.astype(f32)))
    h = h @ w3.astype(f32) + b3.astype(f32)
    deltas = jnp.abs(jnp.linspace(HY_MIN_DECAY, HY_MAX_DECAY, HYENA_W, dtype=f32))
    window = jnp.exp(-t_norm[:, None] * deltas[None, :])
    return h.reshape(L, HY_ORDER, 2, HYENA_W) * window[:, None, None, :]


def bidirectional_long_conv(u, h, bias):
    L, C = h.shape[0], h.shape[-1]
    kern = jnp.concatenate([h[:, 0], jnp.zeros((1, C), h.dtype), h[:0:-1, 1]], axis=0)
    uf = u.astype(jnp.float32)
    spec = jnp.fft.rfft(uf, n=2 * L, axis=1) * jnp.fft.rfft(kern, n=2 * L, axis=0)[None]
    y = jnp.fft.irfft(spec, n=2 * L, axis=1)[:, :L]
    return (y + uf * bias.astype(jnp.float32)).astype(u.dtype)


def hyena(p, short_w, short_b, filt, bias):
    u = depthwise_conv(p, short_w, short_b)
    v, x1, x2 = jnp.split(u, 3, axis=-1)
    z = x1 * bidirectional_long_conv(v, filt[:, 0], bias[0])
    return x2 * bidirectional_long_conv(z, filt[:, 1], bias[1])


def mixer_outputs(p, k_all, v_all, rope, filt, dw_w, dw_b, ln_g, ln_b, q_g, w_uq, sh_w, sh_b, hy_bias):
    conv_out = conformer_conv(p[..., :OFF_Q], dw_w, dw_b, ln_g, ln_b)
    q = mla_q(p[..., OFF_Q:OFF_KV], q_g, w_uq, rope)
    att_out = block_attention(q, k_all, v_all)
    hy_out = hyena(p[..., OFF_HY:], sh_w, sh_b, filt, hy_bias)
    return jnp.concatenate([conv_out, att_out, hy_out], axis=-1)


def moe(h, w_router, router_bias, w_gate, w_up, w_down):
    B, L, D = h.shape
    t = h.reshape(B * L, D)
    scores = jax.nn.sigmoid((t @ w_router).astype(jnp.float32))
    sel = scores + router_bias.astype(jnp.float32)
    gscore = lax.top_k(sel.reshape(-1, N_GROUPS, EXPERTS_PER_GROUP), TOP_K)[0].sum(-1)
    gbest = jnp.argmax(gscore, axis=-1)
    in_group = (jnp.arange(N_EXPERTS) // EXPERTS_PER_GROUP)[None, :] == gbest[:, None]
    _, idx = lax.top_k(jnp.where(in_group, sel, -jnp.inf), TOP_K)
    gsel = jnp.take_along_axis(scores, idx, axis=-1)
    gsel = gsel / jnp.sum(gsel, axis=-1, keepdims=True)
    combine = jnp.sum(jax.nn.one_hot(idx, N_EXPERTS, dtype=jnp.float32) * gsel[..., None], axis=1)
    y = jnp.zeros_like(t)
    for e in range(N_EXPERTS):
        he = jax.nn.silu(t @ w_gate[e]) * (t @ w_up[e])
        y = y + combine[:, e:e + 1].astype(t.dtype) * (he @ w_down[e])
    return y.reshape(B, L, D)


def setup_inputs(seed: int = 0) -> dict:
    key = jax.random.key(seed)
    keys = iter(jax.random.split(key, 40))

    def nrm(shape, scale):
        return scale * jax.random.normal(next(keys), shape, jnp.float32)

    def gain(shape):
        return 1.0 + nrm(shape, 0.02)

    D = D_MODEL
    return {
        "x": nrm((BATCH, SEQ, D), 1.0),
        "c": nrm((BATCH, D), 1.0),
        "ctx": nrm((BATCH, CTX_LEN, D), 1.0),
        "c_ctx": nrm((D,), 1.0),
        "norm1_g": gain((DEPTH, D)),
        "norm2_g": gain((DEPTH, D)),
        "w_ada": nrm((DEPTH, D, 6 * D), 0.5 * D ** -0.5),
        "b_ada": nrm((DEPTH, 6 * D), 0.02),
        "w_in": nrm((DEPTH, D, N_IN), D ** -0.5),
        "conv_dw_w": nrm((DEPTH, CONV_KSIZE, CONV_W), CONV_KSIZE ** -0.5),
        "conv_dw_b": nrm((DEPTH, CONV_W), 0.02),
        "conv_ln_g": gain((DEPTH, CONV_W)),
        "conv_ln_b": nrm((DEPTH, CONV_W), 0.02),
        "q_norm_g": gain((DEPTH, MLA_Q_RANK)),
        "w_uq": nrm((DEPTH, MLA_Q_RANK, MLA_HEADS * (MLA_NOPE + MLA_ROPE)), MLA_Q_RANK ** -0.5),
        "kv_norm_g": gain((DEPTH, MLA_KV_RANK)),
        "w_ukv": nrm((DEPTH, MLA_KV_RANK, MLA_HEADS * (MLA_NOPE + MLA_V_DIM)), MLA_KV_RANK ** -0.5),
        "hy_short_w": nrm((DEPTH, HY_SHORT, (HY_ORDER + 1) * HYENA_W), HY_SHORT ** -0.5),
        "hy_short_b": nrm((DEPTH, (HY_ORDER + 1) * HYENA_W), 0.02),
        "hy_w1": nrm((DEPTH, HY_EMB, HY_FFN), HY_EMB ** -0.5),
        "hy_b1": nrm((DEPTH, HY_FFN), 0.02),
        "hy_freq1": gain((DEPTH, HY_FFN)),
        "hy_w2": nrm((DEPTH, HY_FFN, HY_FFN), HY_FFN ** -0.5),
        "hy_b2": nrm((DEPTH, HY_FFN), 0.02),
        "hy_freq2": gain((DEPTH, HY_FFN)),
        "hy_w3": nrm((DEPTH, HY_FFN, HY_ORDER * 2 * HYENA_W), 0.02),
        "hy_b3": nrm((DEPTH, HY_ORDER * 2 * HYENA_W), 0.01),
        "hy_bias": nrm((DEPTH, HY_ORDER, HYENA_W), 0.1),
        "w_out": nrm((DEPTH, D_MIX, D), D_MIX ** -0.5),
        "w_router": nrm((D, N_EXPERTS), D ** -0.5),
        "router_bias": nrm((N_EXPERTS,), 0.01),
        "w_gate": nrm((DEPTH, N_EXPERTS, D, D_FF_EXPERT), D ** -0.5),
        "w_up": nrm((DEPTH, N_EXPERTS, D, D_FF_EXPERT), D ** -0.5),
        "w_down": nrm((DEPTH, N_EXPERTS, D_FF_EXPERT, D), D_FF_EXPERT ** -0.5),
        "final_norm_g": gain((D,)),
    }


def reference(x, c, ctx, c_ctx, norm1_g, norm2_g, w_ada, b_ada, w_in, conv_dw_w, conv_dw_b, conv_ln_g,
              conv_ln_b, q_norm_g, w_uq, kv_norm_g, w_ukv, hy_short_w, hy_short_b, hy_w1, hy_b1, hy_freq1,
              hy_w2, hy_b2, hy_freq2, hy_w3, hy_b3, hy_bias, w_out, w_router, router_bias, w_gate, w_up,
              w_down, final_norm_g):
    L = x.shape[1]
    Lc = ctx.shape[1]
    rope = axial_rope_tables(L, x.dtype)
    xl, xc = x, ctx
    for i in range(DEPTH):
        last = i == DEPTH - 1
        sl1, scl1, gl1, sl2, scl2, gl2 = modulation(c, w_ada[i], b_ada[i])
        sc1, scc1, gc1, sc2, scc2, gc2 = modulation(c_ctx[None, :], w_ada[i], b_ada[i])

        hl = modulate(rmsnorm(xl, norm1_g[i]), sl1, scl1)
        hc = modulate(rmsnorm(xc, norm1_g[i]), sc1, scc1)
        pl = hl @ w_in[i]
        if last:
            pc_kv = hc @ w_in[i][:, OFF_KV:OFF_HY]
        else:
            pc = hc @ w_in[i]
            pc_kv = pc[..., OFF_KV:OFF_HY]
        kc, vc = mla_kv(pc_kv, kv_norm_g[i], w_ukv[i], None)
        kl, vl = mla_kv(pl[..., OFF_KV:OFF_HY], kv_norm_g[i], w_ukv[i], rope)
        k_all = jnp.concatenate([kc, kl], axis=1)
        v_all = jnp.concatenate([vc, vl], axis=1)

        filt_l = hyena_filters(L, hy_w1[i], hy_b1[i], hy_freq1[i], hy_w2[i], hy_b2[i], hy_freq2[i],
                               hy_w3[i], hy_b3[i])
        ol = mixer_outputs(pl, k_all, v_all, rope, filt_l, conv_dw_w[i], conv_dw_b[i], conv_ln_g[i],
                           conv_ln_b[i], q_norm_g[i], w_uq[i], hy_short_w[i], hy_short_b[i], hy_bias[i])
        xl = xl + gl1 * (ol @ w_out[i])

        if not last:
            filt_c = hyena_filters(Lc, hy_w1[i], hy_b1[i], hy_freq1[i], hy_w2[i], hy_b2[i], hy_freq2[i],
                                   hy_w3[i], hy_b3[i])
            oc = mixer_outputs(pc, kc, vc, None, filt_c, conv_dw_w[i], conv_dw_b[i], conv_ln_g[i],
                               conv_ln_b[i], q_norm_g[i], w_uq[i], hy_short_w[i], hy_short_b[i], hy_bias[i])
            xc = xc + gc1 * (oc @ w_out[i])

        hl2 = modulate(rmsnorm(xl, norm2_g[i]), sl2, scl2)
        xl = xl + gl2 * moe(hl2, w_router, router_bias, w_gate[i], w_up[i], w_down[i])
        if not last:
            hc2 = modulate(rmsnorm(xc, norm2_g[i]), sc2, scc2)
            xc = xc + gc2 * moe(hc2, w_router, router_bias, w_gate[i], w_up[i], w_down[i])

    return rmsnorm(xl, final_norm_g)
```

```python
import math
from contextlib import ExitStack
import numpy as np
import ml_dtypes
import concourse.bass as bass
import concourse.mybir as mybir
from concourse.bass_utils import run_bass_kernel_spmd

F32 = mybir.dt.float32
BF16 = mybir.dt.bfloat16
AF = mybir.ActivationFunctionType
ALU = mybir.AluOpType
AX = mybir.AxisListType

D = 2048
L = 2048
LC = 256
T = L + LC
DEPTH = 2
N_IN = 3904
OFF_Q, OFF_KV, OFF_HY = 1024, 1792, 2368
NH = 8
EPS = 1e-6
NE = 16
DFF = 1024
SCALE = 192 ** -0.5
PI = math.pi


class Tile:
    __slots__ = ("ap", "w", "r", "dsem", "name", "excl")

    def __init__(self, ap, name="", excl=False):
        self.excl = excl
        self.ap = ap
        self.w = None
        self.r = []
        self.dsem = None
        self.name = name

    def __getitem__(self, idx):
        return self.ap[idx]


class Eng:
    def __init__(self, e, sem, name):
        self.e = e
        self.sem = sem
        self.cnt = 0
        self.seen = {}
        self.name = name

    def wait(self, tok):
        if tok is None:
            return
        sem, val = tok
        if self.name == "pe" and sem is self.sem:
            return
        if self.seen.get(sem, 0) >= val:
            return
        self.e.wait_ge(sem, val)
        self.seen[sem] = val


class KB:
    def __init__(self, nc, es, nsem=80):
        self.nc = nc
        self.es = es
        mk = lambda n: es.enter_context(nc.semaphore(n))
        self.pe = Eng(nc.tensor, mk("s_pe"), "pe")
        self.act = Eng(nc.scalar, mk("s_act"), "act")
        self.dve = Eng(nc.vector, mk("s_dve"), "dve")
        self.pool = Eng(nc.gpsimd, mk("s_pool"), "pool")
        self.sp = Eng(nc.sync, mk("s_sp"), "sp")
        self.engs = [self.pe, self.act, self.dve, self.pool, self.sp]
        self.free_sems = [mk(f"s_d{i}") for i in range(nsem)]
        self.semval = {}
        self.pending = []
        self.phase_tiles = []
        self.pes = None
        self.n_inst = 0

    def sb(self, stack, name, shape, dt):
        self.uid = getattr(self, "uid", 0) + 1
        name = f"{name}_{self.uid}"
        t = Tile(stack.enter_context(self.nc.sbuf_tensor(name, list(shape), dt)), name)
        self.phase_tiles.append(t)
        return t

    def sub(self, ap, name=""):
        t = Tile(ap, name)
        self.phase_tiles.append(t)
        return t

    def _dsem(self, t):
        if t.dsem is None:
            assert self.free_sems, "out of DMA semaphores"
            t.dsem = self.free_sems.pop()
        return t.dsem

    def op(self, eng, fn, reads=(), writes=()):
        for t in reads:
            eng.wait(t.w)
            if t.excl:
                for r in t.r:
                    if r[0] is not eng.sem:
                        eng.wait(r)
        for t in writes:
            eng.wait(t.w)
            for r in t.r:
                eng.wait(r)
        ins = fn(eng.e)
        eng.cnt += 1
        ins.then_inc(eng.sem, 1)
        tok = (eng.sem, eng.cnt)
        for t in reads:
            t.r.append(tok)
        for t in writes:
            t.w = tok
            t.r = []
        self.n_inst += 1
        return tok

    def group(self, eng, fns, reads=(), writes=()):
        for t in reads:
            eng.wait(t.w)
            if t.excl:
                for r in t.r:
                    if r[0] is not eng.sem:
                        eng.wait(r)
        for t in writes:
            eng.wait(t.w)
            for r in t.r:
                eng.wait(r)
        ins = None
        for fn in fns:
            ins = fn(eng.e)
        eng.cnt += 1
        ins.then_inc(eng.sem, 1)
        tok = (eng.sem, eng.cnt)
        for t in reads:
            t.r.append(tok)
        for t in writes:
            t.w = tok
            t.r = []
        self.n_inst += len(fns)
        return tok

    def dma(self, q, pairs, dst=None, src=None, **kw):
        if src is not None:
            q.wait(src.w)
        if dst is not None:
            q.wait(dst.w)
            for r in dst.r:
                q.wait(r)
        t = dst if dst is not None else src
        sem = self._dsem(t)
        v = self.semval.get(sem, 0)
        for (o, i) in pairs:
            q.e.dma_start(out=o, in_=i, **kw).then_inc(sem, 16)
            v += 16
        self.semval[sem] = v
        tok = (sem, v)
        if dst is not None:
            dst.w = tok
            dst.r = []
            if src is not None:
                src.r.append(tok)
        else:
            src.r.append(tok)
        self.pending.append(tok)
        self.n_inst += len(pairs)
        return tok

    def barrier(self):
        toks = [(e.sem, e.cnt) for e in self.engs if e.cnt > 0] + self.pending
        best = {}
        for s, v in toks:
            if best.get(s, 0) < v:
                best[s] = v
        for e in self.engs:
            for s, v in best.items():
                if s is e.sem:
                    continue
                e.wait((s, v))
        self.pending = []
        for t in self.phase_tiles:
            if t.dsem is not None:
                self.free_sems.append(t.dsem)
                t.dsem = None
            t.w = None
            t.r = []
        self.phase_tiles = []

    def init_psum(self, stack):
        self.ps = [Tile(stack.enter_context(self.nc.psum_tensor(f"ps{i}", [128, 512], F32)), f"ps{i}", True) for i in range(6)]
        self.psb = [Tile(stack.enter_context(self.nc.psum_tensor(f"psb{i}", [128, 1024], BF16)), f"psb{i}", True) for i in range(2)]
        self.ps_i = 0

    def next_ps(self):
        t = self.ps[self.ps_i % len(self.ps)]
        self.ps_i += 1
        return t


def mm(kb, ps_ap, ps_tile, terms, reads):
    n = len(terms)
    fns = []
    for i, (l, r) in enumerate(terms):
        fns.append(lambda e, l=l, r=r, i=i: e.matmul(ps_ap, lhsT=l, rhs=r, start=(i == 0), stop=(i == n - 1)))
    return kb.group(kb.pe, fns, reads=reads, writes=[ps_tile])


def tr(kb, ps_ap, ps_tile, in_ap, ident_ap, reads):
    return kb.op(kb.pe, lambda e: e.transpose(ps_ap, in_ap, ident_ap), reads=reads, writes=[ps_tile])


def act(kb, out, in_, func, reads, writes, bias=0.0, scale=1.0, accum_out=None):
    kw = {}
    if accum_out is not None:
        kw["accum_out"] = accum_out
    return kb.op(kb.act, lambda e: e.activation(out=out, in_=in_, func=func, bias=bias, scale=scale, **kw),
                 reads=reads, writes=writes)


def ts(kb, eng, out, in0, s1, s2, op0, op1, reads, writes):
    if s2 is None:
        return kb.op(eng, lambda e: e.tensor_scalar(out=out, in0=in0, scalar1=s1, scalar2=None, op0=op0),
                     reads=reads, writes=writes)
    return kb.op(eng, lambda e: e.tensor_scalar(out=out, in0=in0, scalar1=s1, scalar2=s2, op0=op0, op1=op1),
                 reads=reads, writes=writes)


def tt(kb, eng, out, in0, in1, op, reads, writes):
    return kb.op(eng, lambda e: e.tensor_tensor(out=out, in0=in0, in1=in1, op=op), reads=reads, writes=writes)


def stt(kb, eng, out, in0, scalar, in1, op0, op1, reads, writes):
    return kb.op(eng, lambda e: e.scalar_tensor_tensor(out=out, in0=in0, scalar=scalar, in1=in1, op0=op0, op1=op1),
                 reads=reads, writes=writes)


def cp(kb, eng, out, in_, reads, writes):
    if eng is kb.act:
        return kb.op(eng, lambda e: e.copy(out=out, in_=in_), reads=reads, writes=writes)
    return kb.op(eng, lambda e: e.tensor_copy(out=out, in_=in_), reads=reads, writes=writes)


def memset(kb, eng, ap, val, writes):
    return kb.op(eng, lambda e: e.memset(ap, val), reads=(), writes=writes)


def bf(a):
    return np.ascontiguousarray(a).astype(ml_dtypes.bfloat16)


def make_consts():
    c = {}
    c["ident32"] = np.eye(128, dtype=np.float32)
    c["identb"] = bf(np.eye(128, dtype=np.float32))
    c["ones32"] = np.ones((128, 128), np.float32)
    c["onesb"] = bf(np.ones((128, 128), np.float32))
    pos = np.arange(L)
    r = (pos // 64).astype(np.float32)
    col = (pos % 64).astype(np.float32)
    inv = (10000.0 ** (-np.arange(16, dtype=np.float32) / 16)).astype(np.float32)
    ang_r = (r[None, :] * inv[:, None]).astype(np.float32)
    ang_c = (col[None, :] * inv[:, None]).astype(np.float32)
    cos = np.concatenate([np.cos(ang_r), np.cos(ang_r), np.cos(ang_c), np.cos(ang_c)], 0)
    sin = np.concatenate([-np.sin(ang_r), np.sin(ang_r), -np.sin(ang_c), np.sin(ang_c)], 0)
    c["rcos"] = cos.astype(np.float32)
    c["rsin"] = sin.astype(np.float32)
    perm = np.zeros((64, 64), np.float32)
    for m in range(64):
        k = m + 16 if (m % 32) < 16 else m - 16
        perm[k, m] = 1.0
    c["perm"] = bf(perm)
    for Ls, tag in ((L, "l"), (LC, "c")):
        t = np.arange(Ls, dtype=np.float32)
        tn = t / max(Ls - 1, 1)
        bands = np.linspace(1e-4, 15, 16, dtype=np.float32)
        ang = (np.float32(2.0 * math.pi / Ls) * t[:, None] * bands[None, :]).astype(np.float32)
        z = np.concatenate([tn[:, None], np.cos(ang), -np.sin(ang)], -1).astype(np.float32)
        c["hz_" + tag] = np.ascontiguousarray(z.T)
        deltas = np.abs(np.linspace(math.log(1e-2) / 1.5, math.log(1e-2) / 0.3, 512, dtype=np.float32))
        c["hwin_" + tag] = np.exp(-tn[:, None] * deltas[None, :]).astype(np.float32)
        n2 = 2 * Ls
        idx = (np.arange(Ls)[:, None].astype(np.int64) * np.arange(Ls)[None, :].astype(np.int64)) % n2
        a = idx.astype(np.float64) * (2.0 * math.pi / n2)
        c["dftc_" + tag] = bf(np.cos(a))
        c["dfts_" + tag] = bf(np.sin(a))
        alt = np.where(np.arange(Ls) % 2 == 0, 1.0, -1.0).astype(np.float32)
        c["alt_" + tag] = bf(alt[:, None] * np.ones((1, 1), np.float32))
        c["altr_" + tag] = bf(0.5 * alt[None, :])
    return c


def stream(kb, tiles, loads, compute, q, keep=0, **kw):
    n = len(loads)
    R = len(tiles)
    issued = 0
    for j in range(n):
        while issued < min(n, j + R - keep):
            t = tiles[issued % R]
            kb.dma(q, loads[issued](t), dst=t, **kw)
            issued += 1
        compute(j, tiles[j % R])


class NS:
    pass


def phase_params(kb, A, C, li, lst):
    P = NS()
    P.pa = kb.sb(lst, f"pa{li}", [128, 86], F32)
    P.pb = kb.sb(lst, f"pb{li}", [128, 124], F32)
    P.pc = kb.sb(lst, f"pc{li}", [128, 96], F32)
    P.pd = kb.sb(lst, f"pd{li}", [128, 48], F32)
    P.mod = kb.sb(lst, f"mod{li}", [128, 96, 2], F32)
    P.g1m = kb.sb(lst, f"g1m{li}", [128, 16, 2], F32)
    P.g2m = kb.sb(lst, f"g2m{li}", [128, 16, 2], F32)
    with ExitStack() as st:
        stA = kb.sb(st, "stA", [128, 128], F32)
        stB = kb.sb(st, "stB", [128, 128], F32)
        stC = kb.sb(st, "stC", [128, 128], F32)
        stD = kb.sb(st, "stD", [128, 128], F32)
        scT = kb.sb(st, "scT", [128, 2, 16], BF16)
        brow = kb.sb(st, "brow", [2, 4096], F32)
        grow = [kb.sb(st, f"grow{i}", [2, 512], F32) for i in range(2)]
        ring = [kb.sb(st, f"wr{i}", [128, 16, 512], BF16) for i in range(3)]
        q = kb.sp
        rowsA = [(A.c, 16), (A.c_ctx, 16), (A.norm1_g[li], 16), (A.norm2_g[li], 16), (A.conv_dw_b[li], 4),
                 (A.conv_ln_g[li], 4), (A.conv_ln_b[li], 4), (A.q_norm_g[li], 6), (A.kv_norm_g[li], 4)]
        r0 = 0
        pairs = []
        for ap, n in rowsA:
            pairs.append((stA[r0:r0 + n, :], ap))
            r0 += n
        assert r0 == 86
        kb.dma(q, pairs, dst=stA)
        kb.dma(q, [(stB[0:124, :], A.conv_dw_w[li])], dst=stB)
        kb.dma(q, [(stC[0:96, :], A.b_ada[li])], dst=stC)
        kb.dma(q, [(stD[0:36, :], A.hy_short_w[li]), (stD[36:48, :], A.hy_short_b[li])], dst=stD)
        bsrc = A.b_ada_row[li]
        kb.dma(q, [(brow[:, 0:2048], bsrc[:, 4096:6144].partition_broadcast(2)),
                   (brow[:, 2048:4096], bsrc[:, 10240:12288].partition_broadcast(2))], dst=brow)
        for stg, n, dstt in ((stA, 86, P.pa), (stB, 124, P.pb), (stC, 96, P.pc), (stD, 48, P.pd)):
            ps = kb.next_ps()
            tr(kb, ps[:, 0:n], ps, stg[0:n, :], C.ident32[0:n, 0:n], reads=[stg, C.ident32])
            cp(kb, kb.dve, dstt[:, :], ps[:, 0:n], reads=[ps], writes=[dstt])
        act(kb, scT[:, :, :], P.pa[:, 0:32].rearrange("p (s k) -> p s k", s=2), AF.Silu, reads=[P.pa], writes=[scT])

        wv = A.w_ada[li].rearrange("(kc p) n -> p kc n", p=128)
        loads = [(lambda t, j=j: [(t[:, :, :], wv[:, :, j * 512:(j + 1) * 512])]) for j in range(24)]

        def comp(j, slot):
            ps = kb.next_ps()
            for mc in range(4):
                terms = [(slot[:, kc, mc * 128:(mc + 1) * 128], scT[:, :, kc]) for kc in range(16)]
                mm(kb, ps[:, 2 * mc:2 * mc + 2], ps, terms, reads=[slot, scT])
            for mc in range(4):
                jj = j * 4 + mc
                ts(kb, kb.dve, P.mod[:, jj, :], ps[:, 2 * mc:2 * mc + 2], P.pc[:, jj:jj + 1], None, ALU.add, None,
                   reads=[ps, P.pc], writes=[P.mod])
            gsel = {8: 0, 9: 0, 10: 0, 11: 0, 20: 1, 21: 1, 22: 1, 23: 1}
            if j in gsel:
                g = gsel[j]
                blk = j % 4
                ps2 = kb.next_ps()
                terms = [(scT[:, :, kc], slot[:, kc, :]) for kc in range(16)]
                mm(kb, ps2[0:2, :], ps2, terms, reads=[slot, scT])
                gr = grow[j % 2]
                tt(kb, kb.dve, gr[:, :], ps2[0:2, :], brow[:, g * 2048 + blk * 512: g * 2048 + (blk + 1) * 512], ALU.add,
                   reads=[ps2, brow], writes=[gr])
                kb.dma(kb.sp, [(A.gates[li, g, :, blk * 512:(blk + 1) * 512], gr[:, :])], src=gr)

        stream(kb, ring, loads, comp, kb.pool)
        for s in range(2):
            stt(kb, kb.dve, P.g1m[:, :, s], P.mod[:, 16:32, s], 1.0, P.pa[:, 32:48], ALU.add, ALU.mult,
                reads=[P.mod, P.pa], writes=[P.g1m])
            stt(kb, kb.dve, P.g2m[:, :, s], P.mod[:, 64:80, s], 1.0, P.pa[:, 48:64], ALU.add, ALU.mult,
                reads=[P.mod, P.pa], writes=[P.g2m])
        kb.barrier()
    return P


def tok_rstd(kb, xt, junk, ss, rs, width):
    memset(kb, kb.dve, ss[:, 0:1], 0.0, writes=[ss])
    act(kb, junk[:, 0:width], xt[:, 0:width], AF.Square, reads=[xt, ss], writes=[junk, ss], accum_out=ss[:, 0:1])
    act(kb, rs[:, 0:1], ss[:, 0:1], AF.Sqrt, reads=[ss], writes=[rs], bias=EPS, scale=1.0 / width)
    kb.op(kb.dve, lambda e: e.reciprocal(out=rs[:, 0:1], in_=rs[:, 0:1]), reads=[rs], writes=[rs])


def phase_inproj(kb, A, C, P, li, xsrc, xcsrc):
    last = li == DEPTH - 1
    with ExitStack() as st:
        hT = kb.sb(st, "hT", [128, 16, T], BF16)
        hTt = [kb.sub(hT.ap[:, :, i * 128:(i + 1) * 128], f"hT{i}") for i in range(18)]
        xts = [kb.sb(st, f"xt{i}", [128, D], F32) for i in range(2)]
        xns = [kb.sb(st, f"xn{i}", [128, D], BF16) for i in range(2)]
        junk = kb.sb(st, "junk", [128, D], BF16)
        sss = [kb.sb(st, f"ss{i}", [128, 1], F32) for i in range(2)]
        rss = [kb.sb(st, f"rs{i}", [128, 1], F32) for i in range(2)]
        ring = [kb.sb(st, f"wr{i}", [128, 16, 512], BF16) for i in range(3)]
        ots = [kb.sb(st, f"ot{i}", [128, 512], BF16) for i in range(4)]

        def src_rows(ti):
            return xsrc[ti * 128:(ti + 1) * 128, :] if ti < 16 else xcsrc[(ti - 16) * 128:(ti - 15) * 128, :]

        loads = [(lambda t, ti=ti: [(t[:, :], src_rows(ti))]) for ti in range(18)]
        cnt = [0]

        def comp(ti, xt):
            s = 0 if ti < 16 else 1
            xn, ss, rs = xns[ti % 2], sss[ti % 2], rss[ti % 2]
            tok_rstd(kb, xt, junk, ss, rs, D)
            ts(kb, kb.dve, xn[:, :], xt[:, :], rs[:, 0:1], None, ALU.mult, None, reads=[xt, rs], writes=[xn])
            for g in range(2):
                ph = kb.psb[g]
                for k8 in range(8):
                    kc = g * 8 + k8
                    tr(kb, ph[:, k8 * 128:(k8 + 1) * 128], ph, xn[:, kc * 128:(kc + 1) * 128], C.identb[:, :],
                       reads=[xn, C.identb])
                for k8 in range(8):
                    kc = g * 8 + k8
                    o = hTt[ti][:, kc, :]
                    i_ = ph[:, k8 * 128:(k8 + 1) * 128]
                    if g == 0:
                        ts(kb, kb.dve, o, i_, P.g1m[:, kc, s:s + 1], P.mod[:, kc, s:s + 1], ALU.mult, ALU.add,
                           reads=[ph, P.g1m, P.mod], writes=[hTt[ti]])
                    else:
                        act(kb, o, i_, AF.Identity, reads=[ph, P.g1m, P.mod], writes=[hTt[ti]],
                            bias=P.mod[:, kc, s:s + 1], scale=P.g1m[:, kc, s:s + 1])

        import os
        if not os.environ.get('K_SKIP_NORM'):
            stream(kb, xts, loads, comp, kb.sp)

        wv = A.w_in[li].rearrange("(kc p) n -> p kc n", p=128)
        pieces = [(j * 512, min(512, N_IN - j * 512)) for j in range(8)]
        loads = [(lambda t, c0=c0, w=w: [(t[:, :, 0:w], wv[:, :, c0:c0 + w])]) for (c0, w) in pieces]
        oc = [0]

        npc = int(os.environ.get('K_PIECES', '8'))
        pieces = pieces[:npc]
        loads = loads[:npc]
        evac_mode = os.environ.get('K_EVAC', 'both')

        def comp2(j, slot):
            c0, w = pieces[j]
            tbs = [(tb * 512, 512) for tb in range(4)]
            if (not last) or j in (3, 4):
                tbs.append((2048, 256))
            for (t0, n) in tbs:
                rd = [hTt[t0 // 128 + i] for i in range(n // 128)]
                for m0 in ([0, 128, 256, 384] if w == 512 else [0, 128, w - 128]):
                    mw = 128
                    ps = kb.next_ps()
                    terms = [(slot[:, kc, m0:m0 + mw], hT[:, kc, t0:t0 + n]) for kc in range(16)]
                    mm(kb, ps[0:mw, 0:n], ps, terms, reads=[slot] + rd)
                    ot = ots[oc[0] % 4]
                    cp(kb, (kb.act if oc[0] % 2 else kb.dve) if evac_mode == 'both' else kb.dve, ot[0:mw, 0:n], ps[0:mw, 0:n], reads=[ps], writes=[ot])
                    oc[0] += 1
                    kb.dma(kb.sp, [(A.pT[c0 + m0:c0 + m0 + mw, t0:t0 + n], ot[0:mw, 0:n])], src=ot)

        if not os.environ.get('K_SKIP_PROJ'):
            stream(kb, ring, loads, comp2, kb.pool)
        kb.barrier()


IN_SPECS = [
    ("x", [L, D], F32), ("c", [16, 128], F32), ("ctx", [LC, D], F32), ("c_ctx", [16, 128], F32),
    ("norm1_g", [2, 16, 128], F32), ("norm2_g", [2, 16, 128], F32),
    ("w_ada", [2, D, 6 * D], F32), ("b_ada", [2, 96, 128], F32), ("b_ada_row", [2, 1, 6 * D], F32),
    ("w_in", [2, D, N_IN], F32),
    ("conv_dw_w", [2, 124, 128], F32), ("conv_dw_b", [2, 4, 128], F32), ("conv_ln_g", [2, 4, 128], F32),
    ("conv_ln_b", [2, 4, 128], F32), ("q_norm_g", [2, 6, 128], F32), ("w_uq", [2, 768, 1536], F32),
    ("kv_norm_g", [2, 4, 128], F32), ("w_ukv", [2, 512, 2048], F32),
    ("hy_short_w", [2, 36, 128], F32), ("hy_short_b", [2, 12, 128], F32),
    ("hy_w1", [2, 33, 64], F32), ("hy_b1", [2, 64, 1], F32), ("hy_freq1", [2, 64, 1], F32),
    ("hy_w2", [2, 64, 64], F32), ("hy_b2", [2, 64, 1], F32), ("hy_freq2", [2, 64, 1], F32),
    ("hy_w3", [2, 64, 2048], F32), ("hy_b3", [2, 1, 2048], F32), ("hy_bias", [2, 2, 512], F32),
    ("w_out", [2, D, D], F32), ("w_router", [D, NE], F32), ("router_bias", [1, NE], F32),
    ("w_gate", [2, NE, D, DFF], F32), ("w_up", [2, NE, D, DFF], F32), ("w_down", [2, NE, DFF, D], F32),
    ("final_norm_g", [1, D], F32),
]
CONST_DT = {"identb": BF16, "onesb": BF16, "perm": BF16, "dftc_l": BF16, "dfts_l": BF16, "dftc_c": BF16,
            "dfts_c": BF16, "alt_l": BF16, "alt_c": BF16, "altr_l": BF16, "altr_c": BF16}
SCRATCH = [
    ("gates", [2, 2, 2, D], F32),
    ("pT", [N_IN, T], BF16),
    ("olT", [D, T], BF16),
    ("kTn", [NH, 128, T], BF16), ("krT", [128, T], BF16), ("vtm", [T, NH * 128], BF16),
    ("qTn", [NH, 128, T], BF16), ("qTr", [NH, 128, T], BF16), ("uTd", [1536, T], BF16),
    ("xs", [L, D], F32), ("xcs", [LC, D], F32), ("h2Td", [D, T], BF16), ("combd", [T, NE], F32),
]


def build(consts, dbg=(), stop_after=None, skip=()):
    nc = bass.Bass("TRN2", target_bir_lowering=False)
    A = NS()
    for name, shape, dt in IN_SPECS:
        if name in skip:
            continue
        setattr(A, name, nc.dram_tensor(name, shape, dt, kind="ExternalInput").ap())
    Cd = NS()
    for name, arr in consts.items():
        setattr(Cd, name, nc.dram_tensor("k_" + name, list(arr.shape), CONST_DT.get(name, F32), kind="ExternalInput").ap())
    for name, shape, dt in SCRATCH:
        kind = "ExternalOutput" if name in dbg else "Internal"
        setattr(A, name, nc.dram_tensor(name, shape, dt, kind=kind).ap())
    A.out = nc.dram_tensor("out", [L, D], F32, kind="ExternalOutput").ap()

    with ExitStack() as es:
        kb = KB(nc, es)
        kb.init_psum(es)
        C = NS()
        C.ident32 = kb.sb(es, "ident32", [128, 128], F32)
        C.identb = kb.sb(es, "identb", [128, 128], BF16)
        C.ones32 = kb.sb(es, "ones32", [128, 128], F32)
        C.onesb = kb.sb(es, "onesb", [128, 128], BF16)
        C.perm = kb.sb(es, "perm", [64, 64], BF16)
        for nm in ("ident32", "identb", "ones32", "onesb", "perm"):
            t = getattr(C, nm)
            kb.dma(kb.sp, [(t[:, :], getattr(Cd, nm))], dst=t)
        C.d = Cd
        kb.barrier()
        xsrc, xcsrc = A.x, A.ctx
        done = False
        for li in range(DEPTH):
            with ExitStack() as lst:
                P = phase_params(kb, A, C, li, lst)
                if stop_after == ("params", li):
                    done = True
                    break
                phase_inproj(kb, A, C, P, li, xsrc, xcsrc)
                if stop_after == ("inproj", li):
                    done = True
                    break
                import os
                skipm = os.environ.get("K_SKIPM", "")
                if "c" not in skipm:
                    phase_conv(kb, A, C, P, li)
                if "a" not in skipm:
                    phase_qkv(kb, A, C, P, li)
                    phase_attn(kb, A, C, P, li)
                if "h" not in skipm:
                    phase_hyena(kb, A, C, P, li, 0, L, "l")
                    if li < DEPTH - 1:
                        phase_hyena(kb, A, C, P, li, L, LC, "c")
                if stop_after == ("mix", li):
                    done = True
                    break
                phase_outproj(kb, A, C, P, li, xsrc, xcsrc)
                xsrc, xcsrc = A.xs, A.xcs
                if stop_after == ("outproj", li):
                    done = True
                    break
                phase_moe(kb, A, C, P, li)
                if stop_after == ("moe", li):
                    done = True
                    break
        kb.barrier()
        print("instructions:", kb.n_inst)
    return nc


def prep_inputs(inputs, consts, b):
    f = lambda a: np.ascontiguousarray(np.asarray(a, dtype=np.float32))
    g = inputs
    m = {
        "x": f(g["x"][b]), "c": f(g["c"][b]).reshape(16, 128), "ctx": f(g["ctx"][b]),
        "c_ctx": f(g["c_ctx"]).reshape(16, 128),
        "norm1_g": f(g["norm1_g"]).reshape(2, 16, 128), "norm2_g": f(g["norm2_g"]).reshape(2, 16, 128),
        "w_ada": f(g["w_ada"]), "b_ada": f(g["b_ada"]).reshape(2, 96, 128), "b_ada_row": f(g["b_ada"]).reshape(2, 1, 6 * D),
        "w_in": f(g["w_in"]),
        "conv_dw_w": f(g["conv_dw_w"]).reshape(2, 124, 128), "conv_dw_b": f(g["conv_dw_b"]).reshape(2, 4, 128),
        "conv_ln_g": f(g["conv_ln_g"]).reshape(2, 4, 128), "conv_ln_b": f(g["conv_ln_b"]).reshape(2, 4, 128),
        "q_norm_g": f(g["q_norm_g"]).reshape(2, 6, 128), "w_uq": f(g["w_uq"]),
        "kv_norm_g": f(g["kv_norm_g"]).reshape(2, 4, 128), "w_ukv": f(g["w_ukv"]),
        "hy_short_w": f(g["hy_short_w"]).reshape(2, 36, 128), "hy_short_b": f(g["hy_short_b"]).reshape(2, 12, 128),
        "hy_w1": f(g["hy_w1"]), "hy_b1": f(g["hy_b1"]).reshape(2, 64, 1), "hy_freq1": f(g["hy_freq1"]).reshape(2, 64, 1),
        "hy_w2": f(g["hy_w2"]), "hy_b2": f(g["hy_b2"]).reshape(2, 64, 1), "hy_freq2": f(g["hy_freq2"]).reshape(2, 64, 1),
        "hy_w3": f(g["hy_w3"]), "hy_b3": f(g["hy_b3"]).reshape(2, 1, 2048), "hy_bias": f(g["hy_bias"]),
        "w_out": f(g["w_out"]), "w_router": f(g["w_router"]), "router_bias": f(g["router_bias"]).reshape(1, NE),
        "w_gate": f(g["w_gate"]), "w_up": f(g["w_up"]), "w_down": f(g["w_down"]),
        "final_norm_g": f(g["final_norm_g"]).reshape(1, D),
    }
    for k, v in consts.items():
        m["k_" + k] = v
    return m


_CACHE = {}


def kernel(**inputs):
    consts = _CACHE.get("consts")
    if consts is None:
        consts = _CACHE["consts"] = make_consts()
    nc = build(consts)
    in_maps = [prep_inputs(inputs, consts, b) for b in range(8)]
    res = run_bass_kernel_spmd(nc, in_maps, core_ids=list(range(8)))
    return np.stack([np.asarray(r["out"], dtype=np.float32) for r in res.results], axis=0)


def phase_outproj(kb, A, C, P, li, xsrc, xcsrc):
    last = li == DEPTH - 1
    ntile = 16 if last else 18
    BIG = 1.0e4
    with ExitStack() as st:
        wout = kb.sb(st, "wout", [128, 16, D], BF16)
        wv = A.w_out[li].rearrange("(kc p) n -> p kc n", p=128)
        wsubs = []
        for q4 in range(4):
            sub_ = kb.sub(wout.ap[:, q4 * 4:(q4 + 1) * 4, :], f"wout{q4}")
            kb.dma(kb.pool, [(sub_[:, :, :], wv[:, q4 * 4:(q4 + 1) * 4, :])], dst=sub_)
            wsubs.append(sub_)
        gbc = [kb.sb(st, f"gbc{s}", [128, D], F32) for s in range(2)]
        for s in range(2):
            kb.dma(kb.sp, [(gbc[s][:, :], A.gates[li, 0, s:s + 1, :].partition_broadcast(128))], dst=gbc[s])
        wr = kb.sb(st, "wr", [128, 16, NE], F32)
        kb.dma(kb.sp, [(wr[:, :, :], A.w_router.rearrange("(kc p) e -> p kc e", p=128))], dst=wr)
        rb = kb.sb(st, "rb", [128, NE], F32)
        kb.dma(kb.sp, [(rb[:, :], A.router_bias.partition_broadcast(128))], dst=rb)
        ols = [kb.sb(st, f"ol{i}", [128, 16, 512], BF16) for i in range(2)]
        xts = [kb.sb(st, f"xt{i}", [128, D], F32) for i in range(2)]
        xos = [kb.sb(st, f"xo{i}", [128, D], F32) for i in range(2)]
        xns = [kb.sb(st, f"xn{i}", [128, D], F32) for i in range(2)]
        h32s = [kb.sb(st, f"h32{i}", [128, 16, 128], F32) for i in range(2)]
        hbs = [kb.sb(st, f"hb{i}", [128, 16, 128], BF16) for i in range(2)]
        cbs = [kb.sb(st, f"cb{i}", [128, NE], F32) for i in range(2)]
        junk = kb.sb(st, "junk", [128, D], BF16)
        sss = [kb.sb(st, f"ss{i}", [128, 1], F32) for i in range(2)]
        rss = [kb.sb(st, f"rs{i}", [128, 1], F32) for i in range(2)]
        r_sc = kb.sb(st, "r_sc", [128, NE], F32)
        r_sel = kb.sb(st, "r_sel", [128, NE], F32)
        r_t = kb.sb(st, "r_t", [128, NE], F32)
        r_m = kb.sb(st, "r_m", [128, 8], F32)
        r_g = kb.sb(st, "r_g", [128, 8], F32)
        olv = A.olT.rearrange("(kc p) t -> p kc t", p=128)
        h2v = A.h2Td.rearrange("(kc p) t -> p kc t", p=128)

        def xrows(ti):
            return xsrc[ti * 128:(ti + 1) * 128, :] if ti < 16 else xcsrc[(ti - 16) * 128:(ti - 15) * 128, :]

        def load_ol(b):
            t0_ = b * 4
            nbt = min(4, ntile - t0_)
            o = ols[b % 2]
            kb.dma(kb.sp, [(o[:, :, 0:nbt * 128], olv[:, :, t0_ * 128:(t0_ + nbt) * 128])], dst=o)

        def load_x(ti):
            kb.dma(kb.sp, [(xts[ti % 2][:, :], xrows(ti))], dst=xts[ti % 2])

        load_ol(0)
        load_x(0)

        def stage1(ti):
            s = 0 if ti < 16 else 1
            b = ti // 4
            if ti % 4 == 0 and (b + 1) * 4 < ntile:
                load_ol(b + 1)
            if ti + 1 < ntile:
                load_x(ti + 1)
            ol, xt, xo, xn = ols[b % 2], xts[ti % 2], xos[ti % 2], xns[ti % 2]
            h32, hb, cb, ss, rs = h32s[ti % 2], hbs[ti % 2], cbs[ti % 2], sss[ti % 2], rss[ti % 2]
            c0 = (ti % 4) * 128
            orow = A.xs[ti * 128:(ti + 1) * 128, :] if ti < 16 else A.xcs[(ti - 16) * 128:(ti - 15) * 128, :]
            for db in range(4):
                ps = kb.next_ps()
                terms = [(ol[:, kc, c0:c0 + 128], wout[:, kc, db * 512:(db + 1) * 512]) for kc in range(16)]
                mm(kb, ps[:, :], ps, terms, reads=[ol] + wsubs)
                tt(kb, kb.dve, xo[:, db * 512:(db + 1) * 512], ps[:, :], gbc[s][:, db * 512:(db + 1) * 512], ALU.mult,
                   reads=[ps, gbc[s]], writes=[xo])
            tt(kb, kb.dve, xo[:, :], xo[:, :], xt[:, :], ALU.add, reads=[xo, xt], writes=[xo])
            kb.dma(kb.pool, [(orow, xo[:, :])], src=xo)
            tok_rstd(kb, xo, junk, ss, rs, D)
            ts(kb, kb.dve, xn[:, :], xo[:, :], rs[:, 0:1], None, ALU.mult, None, reads=[xo, rs], writes=[xn])

        def stage2(ti):
            s = 0 if ti < 16 else 1
            xn = xns[ti % 2]
            h32, hb, cb = h32s[ti % 2], hbs[ti % 2], cbs[ti % 2]
            for g in range(4):
                ps = kb.next_ps()
                for k4 in range(4):
                    kc = g * 4 + k4
                    tr(kb, ps[:, k4 * 128:(k4 + 1) * 128], ps, xn[:, kc * 128:(kc + 1) * 128], C.ident32[:, :],
                       reads=[xn, C.ident32])
                for k4 in range(4):
                    kc = g * 4 + k4
                    if g % 2 == 0:
                        ts(kb, kb.dve, h32[:, kc, :], ps[:, k4 * 128:(k4 + 1) * 128], P.g2m[:, kc, s:s + 1],
                           P.mod[:, 48 + kc, s:s + 1], ALU.mult, ALU.add, reads=[ps, P.g2m, P.mod], writes=[h32])
                    else:
                        act(kb, h32[:, kc, :], ps[:, k4 * 128:(k4 + 1) * 128], AF.Identity, reads=[ps, P.g2m, P.mod], writes=[h32],
                            bias=P.mod[:, 48 + kc, s:s + 1], scale=P.g2m[:, kc, s:s + 1])
            cp(kb, kb.act, hb[:, :, :], h32[:, :, :], reads=[h32], writes=[hb])
            kb.dma(kb.sp, [(h2v[:, :, ti * 128:(ti + 1) * 128], hb[:, :, :])], src=hb)
        def stage3(ti):
            s = 0 if ti < 16 else 1
            h32, cb = h32s[ti % 2], cbs[ti % 2]
            ps = kb.next_ps()
            mm(kb, ps[:, 0:NE], ps, [(h32[:, kc, :], wr[:, kc, :]) for kc in range(16)], reads=[h32, wr])
            act(kb, r_sc[:, :], ps[:, 0:NE], AF.Sigmoid, reads=[ps], writes=[r_sc])
            tt(kb, kb.dve, r_sel[:, :], r_sc[:, :], rb[:, :], ALU.add, reads=[r_sc, rb], writes=[r_sel])
            sel3 = r_sel[:, :].rearrange("p (g j) -> p g j", g=4)
            t3 = r_t[:, :].rearrange("p (g j) -> p g j", g=4)
            kb.op(kb.dve, lambda e: e.tensor_reduce(out=r_m[:, 0:4], in_=sel3, axis=AX.X, op=ALU.max),
                  reads=[r_sel], writes=[r_m])
            for g in range(4):
                ts(kb, kb.dve, t3[:, g, :], sel3[:, g, :], r_m[:, g:g + 1], -BIG, ALU.is_ge, ALU.mult,
                   reads=[r_sel, r_m], writes=[r_t])
            tt(kb, kb.dve, r_t[:, :], r_t[:, :], r_sel[:, :], ALU.add, reads=[r_t, r_sel], writes=[r_t])
            kb.op(kb.dve, lambda e: e.tensor_reduce(out=r_m[:, 4:8], in_=t3, axis=AX.X, op=ALU.max),
                  reads=[r_t], writes=[r_m])
            tt(kb, kb.dve, r_g[:, 0:4], r_m[:, 0:4], r_m[:, 4:8], ALU.add, reads=[r_m], writes=[r_g])
            kb.op(kb.dve, lambda e: e.tensor_reduce(out=r_g[:, 4:5], in_=r_g[:, 0:4], axis=AX.X, op=ALU.max),
                  reads=[r_g], writes=[r_g])
            ts(kb, kb.dve, r_g[:, 0:4], r_g[:, 0:4], r_g[:, 4:5], None, ALU.is_ge, None, reads=[r_g], writes=[r_g])
            for g in range(4):
                ts(kb, kb.dve, t3[:, g, :], sel3[:, g, :], r_g[:, g:g + 1], None, ALU.mult, None,
                   reads=[r_sel, r_g], writes=[r_t])
                ts(kb, kb.dve, t3[:, g, :], t3[:, g, :], r_g[:, g:g + 1], None, ALU.add, None,
                   reads=[r_t, r_g], writes=[r_t])
            kb.op(kb.dve, lambda e: e.tensor_reduce(out=r_m[:, 0:1], in_=r_t[:, :], axis=AX.X, op=ALU.max),
                  reads=[r_t], writes=[r_m])
            ts(kb, kb.dve, r_sel[:, :], r_t[:, :], r_m[:, 0:1], -BIG, ALU.is_ge, ALU.mult, reads=[r_t, r_m], writes=[r_sel])
            tt(kb, kb.dve, r_sel[:, :], r_sel[:, :], r_t[:, :], ALU.add, reads=[r_sel, r_t], writes=[r_sel])
            kb.op(kb.dve, lambda e: e.tensor_reduce(out=r_m[:, 1:2], in_=r_sel[:, :], axis=AX.X, op=ALU.max),
                  reads=[r_sel], writes=[r_m])
            ts(kb, kb.dve, r_t[:, :], r_t[:, :], r_m[:, 1:2], None, ALU.is_ge, None, reads=[r_t, r_m], writes=[r_t])
            tt(kb, kb.dve, r_t[:, :], r_t[:, :], r_sc[:, :], ALU.mult, reads=[r_t, r_sc], writes=[r_t])
            kb.op(kb.dve, lambda e: e.tensor_reduce(out=r_m[:, 2:3], in_=r_t[:, :], axis=AX.X, op=ALU.add),
                  reads=[r_t], writes=[r_m])
            kb.op(kb.dve, lambda e: e.reciprocal(out=r_m[:, 2:3], in_=r_m[:, 2:3]), reads=[r_m], writes=[r_m])
            ts(kb, kb.dve, cb[:, :], r_t[:, :], r_m[:, 2:3], None, ALU.mult, None, reads=[r_t, r_m], writes=[cb])
            kb.dma(kb.sp, [(A.combd[ti * 128:(ti + 1) * 128, :], cb[:, :])], src=cb)

        for ti in range(ntile + 2):
            if ti < ntile:
                stage1(ti)
            if 1 <= ti <= ntile:
                stage2(ti - 1)
            if ti >= 2:
                stage3(ti - 2)
        kb.barrier()


def phase_moe(kb, A, C, P, li):
    last = li == DEPTH - 1
    ntile = 16 if last else 18
    blocks = []
    t0 = 0
    sizes = [9, 9] if ntile == 18 else [8, 8]
    NBM = max(sizes)
    for nb in sizes:
        blocks.append((t0, nb))
        t0 += nb
    assert t0 == ntile
    BIG = 1.0e4
    with ExitStack() as st:
        gbc = [kb.sb(st, f"gbc{s}", [128, D], F32) for s in range(2)]
        for s in range(2):
            kb.dma(kb.sp, [(gbc[s][:, :], A.gates[li, 1, s:s + 1, :].partition_broadcast(128))], dst=gbc[s])
        fg = None
        if last:
            fg = kb.sb(st, "fg", [128, D], F32)
            kb.dma(kb.sp, [(fg[:, :], A.final_norm_g.partition_broadcast(128))], dst=fg)
        h2T = kb.sb(st, "h2T", [128, 16, NBM * 128], BF16)
        heT = [kb.sb(st, "heT0", [128, 8, NBM * 128], BF16)] * 2
        yacc = kb.sb(st, "yacc", [128, NBM, D], F32)
        yat = [kb.sub(yacc.ap[:, i, :], f"ya{i}") for i in range(NBM)]
        comb = kb.sb(st, "comb", [128, NBM, NE], F32)
        ring = [kb.sb(st, f"wr{i}", [128, 16, 256], BF16) for i in range(4)]
        xt = kb.sb(st, "xt", [128, D], F32)
        xn = kb.sb(st, "xn", [128, D], F32)
        junk = kb.sb(st, "junk", [128, D], BF16)
        ss = kb.sb(st, "ss", [128, 1], F32)
        rs = kb.sb(st, "rs", [128, 1], F32)
        sg = [kb.sb(st, f"sg{i}", [128, 512], F32) for i in range(2)]

        def xrows(ti, src=True):
            if ti < 16:
                base = (A.xs if True else None)
                return A.xs[ti * 128:(ti + 1) * 128, :]
            return A.xcs[(ti - 16) * 128:(ti - 15) * 128, :]

        for (tb0, nb) in blocks:
            ntok = nb * 128
            h2v = A.h2Td.rearrange("(kc p) t -> p kc t", p=128)
            kb.dma(kb.sp, [(h2T[:, :, 0:ntok], h2v[:, :, tb0 * 128:tb0 * 128 + ntok])], dst=h2T)
            kb.dma(kb.sp, [(comb[:, i, :], A.combd[(tb0 + i) * 128:(tb0 + i + 1) * 128, :]) for i in range(nb)], dst=comb)
            nch = (ntok + 511) // 512
            csz = ntok // nch
            nsub = [(o, csz) for o in range(0, ntok, csz)]
            loads = []
            meta = []
            for e in range(NE):
                for f4 in range(4):
                    wg = A.w_gate[li, e].rearrange("(kc p) n -> p kc n", p=128)[:, :, f4 * 256:(f4 + 1) * 256]
                    wu = A.w_up[li, e].rearrange("(kc p) n -> p kc n", p=128)[:, :, f4 * 256:(f4 + 1) * 256]
                    loads.append(lambda t, wg=wg: [(t[:, :, :], wg)])
                    meta.append(("g", e, f4))
                    loads.append(lambda t, wu=wu: [(t[:, :, :], wu)])
                    meta.append(("u", e, f4))
                for d4 in range(4):
                    wd = A.w_down[li, e].rearrange("(fc p) n -> p fc n", p=128)[:, :, d4 * 512:(d4 + 1) * 512]
                    loads.append(lambda t, wd=wd: [(t[:, :, :].rearrange("p a b -> p (a b)").rearrange("p (f n) -> p f n", f=8), wd)])
                    meta.append(("d", e, d4))
            state = {}
            sgc = [0]

            def comp(j, slot):
                kind, e, idx = meta[j]
                he = heT[e % 2]
                if kind == "g":
                    state["g"] = slot
                    return
                if kind == "u":
                    gs = state["g"]
                    for (o, n) in nsub:
                        for m in range(2):
                            ffc = idx * 2 + m
                            psg = kb.next_ps()
                            mm(kb, psg[:, 0:n], psg, [(gs[:, kc, m * 128:(m + 1) * 128], h2T[:, kc, o:o + n]) for kc in range(16)],
                               reads=[gs, h2T])
                            psu = kb.next_ps()
                            mm(kb, psu[:, 0:n], psu, [(slot[:, kc, m * 128:(m + 1) * 128], h2T[:, kc, o:o + n]) for kc in range(16)],
                               reads=[slot, h2T])
                            sgt = sg[sgc[0] % 2]
                            sgc[0] += 1
                            act(kb, sgt[:, 0:n], psg[:, 0:n], AF.Silu, reads=[psg], writes=[sgt])
                            tt(kb, kb.dve, he[:, ffc, o:o + n], psu[:, 0:n], sgt[:, 0:n], ALU.mult, reads=[psu, sgt], writes=[he])
                    return
                wdv = slot[:, :, :].rearrange("p a b -> p (a b)").rearrange("p (f n) -> p f n", f=8)
                for i in range(nb):
                    ps = kb.next_ps()
                    mm(kb, ps[:, :], ps, [(he[:, fc, i * 128:(i + 1) * 128], wdv[:, fc, :]) for fc in range(8)], reads=[he, slot])
                    o_ = yat[i][:, idx * 512:(idx + 1) * 512]
                    if e == 0:
                        ts(kb, kb.dve, o_, ps[:, :], comb[:, i, e:e + 1], None, ALU.mult, None, reads=[ps, comb], writes=[yat[i]])
                    else:
                        stt(kb, kb.dve, o_, ps[:, :], comb[:, i, e:e + 1], o_, ALU.mult, ALU.add, reads=[ps, comb, yat[i]], writes=[yat[i]])

            stream(kb, ring, loads, comp, kb.pool, keep=1)
            for i in range(nb):
                ti = tb0 + i
                s = 0 if ti < 16 else 1
                kb.dma(kb.sp, [(xt[:, :], xrows(ti))], dst=xt)
                tt(kb, kb.dve, xn[:, :], yat[i][:, :], gbc[s][:, :], ALU.mult, reads=[yat[i], gbc[s]], writes=[xn])
                tt(kb, kb.dve, xn[:, :], xn[:, :], xt[:, :], ALU.add, reads=[xn, xt], writes=[xn])
                if last:
                    tok_rstd(kb, xn, junk, ss, rs, D)
                    stt(kb, kb.dve, xn[:, :], xn[:, :], rs[:, 0:1], fg[:, :], ALU.mult, ALU.mult, reads=[xn, rs, fg], writes=[xn])
                    kb.dma(kb.sp, [(A.out[ti * 128:(ti + 1) * 128, :], xn[:, :])], src=xn)
                else:
                    kb.dma(kb.sp, [(xrows(ti), xn[:, :])], src=xn)
        kb.barrier()


def fm_rstd(kb, C, src, nk, ncols, width, rstd, sq):
    for c0 in range(0, ncols, 512):
        n = min(512, ncols - c0)
        for kc in range(nk):
            act(kb, sq[:, kc, 0:n], src[:, kc, c0:c0 + n], AF.Square, reads=[src], writes=[sq])
        ps = kb.next_ps()
        mm(kb, ps[:, 0:n], ps, [(C.onesb[:, :], sq[:, kc, 0:n]) for kc in range(nk)], reads=[C.onesb, sq])
        act(kb, rstd[:, c0:c0 + n], ps[:, 0:n], AF.Sqrt, reads=[ps], writes=[rstd], bias=EPS, scale=1.0 / width)
        kb.op(kb.dve, lambda e, c0=c0, n=n: e.reciprocal(out=rstd[:, c0:c0 + n], in_=rstd[:, c0:c0 + n]), reads=[rstd], writes=[rstd])


def rope_apply(kb, C, a_sb, n, c0, rcos, rsin, out_ap, out_tile, tmp1, tmp2):
    ps = kb.next_ps()
    mm(kb, ps[0:64, 0:n], ps, [(C.perm[:, :], a_sb[0:64, 0:n])], reads=[C.perm, a_sb])
    tt(kb, kb.dve, tmp1[0:64, 0:n], a_sb[0:64, 0:n], rcos[0:64, c0:c0 + n], ALU.mult, reads=[a_sb, rcos], writes=[tmp1])
    tt(kb, kb.dve, tmp2[0:64, 0:n], ps[0:64, 0:n], rsin[0:64, c0:c0 + n], ALU.mult, reads=[ps, rsin], writes=[tmp2])
    tt(kb, kb.dve, out_ap, tmp1[0:64, 0:n], tmp2[0:64, 0:n], ALU.add, reads=[tmp1, tmp2], writes=[out_tile])


TBLK = [(0, 512), (512, 512), (1024, 512), (1536, 512), (2048, 256)]


def phase_qkv(kb, A, C, P, li):
    last = li == DEPTH - 1
    with ExitStack() as st:
        rcos = kb.sb(st, "rcos", [64, L], F32)
        rsin = kb.sb(st, "rsin", [64, L], F32)
        kb.dma(kb.sp, [(rcos[:, :], C.d.rcos)], dst=rcos)
        kb.dma(kb.sp, [(rsin[:, :], C.d.rsin)], dst=rsin)
        rstd = kb.sb(st, "rstd", [128, T], F32)
        sq = kb.sb(st, "sq", [128, 6, 512], BF16)
        tmp1s = [kb.sb(st, f"tmp1{i}", [64, 512], F32) for i in range(2)]
        tmp2s = [kb.sb(st, f"tmp2{i}", [64, 512], F32) for i in range(2)]
        asbs = [kb.sb(st, f"asb{i}", [64, 512], BF16) for i in range(2)]
        rcnt = [0]

        def rope_bufs():
            i = rcnt[0] % 2
            rcnt[0] += 1
            return asbs[i], tmp1s[i], tmp2s[i]
        ots = [kb.sb(st, f"ot{i}", [128, 512], BF16) for i in range(4)]
        oc = [0]

        def evac_store(ps, n, dram_ap):
            ot = ots[oc[0] % 4]
            cp(kb, kb.act if oc[0] % 2 else kb.dve, ot[:, 0:n], ps[:, 0:n], reads=[ps], writes=[ot])
            oc[0] += 1
            kb.dma(kb.sp, [(dram_ap, ot[:, 0:n])], src=ot)

        with ExitStack() as s2:
            pk = kb.sb(s2, "pk", [128, 4, T], BF16)
            kr = kb.sb(s2, "kr", [64, T], BF16)
            ckv = kb.sb(s2, "ckv", [128, 4, T], BF16)
            wk = kb.sb(s2, "wk", [128, 4, NH, 128], BF16)
            wvv = kb.sb(s2, "wvv", [128, 4, NH, 128], BF16)
            kro = kb.sb(s2, "kro", [128, T], BF16)
            kb.dma(kb.sp, [(pk[:, :, :], A.pT[OFF_KV:OFF_KV + 512, :].rearrange("(kc p) t -> p kc t", p=128))], dst=pk)
            kb.dma(kb.sp, [(kr[:, :], A.pT[OFF_KV + 512:OFF_KV + 576, :])], dst=kr)
            wsrc = A.w_ukv[li].rearrange("(kc p) (h t d) -> p kc h t d", p=128, h=NH, t=2)
            kb.dma(kb.pool, [(wk[:, kc, :, :], wsrc[:, kc, :, 0, :]) for kc in range(4)], dst=wk)
            kb.dma(kb.pool, [(wvv[:, kc, :, :], wsrc[:, kc, :, 1, :]) for kc in range(4)], dst=wvv)
            fm_rstd(kb, C, pk, 4, T, 512, rstd, sq)
            for kc in range(4):
                stt(kb, kb.dve, ckv[:, kc, :], pk[:, kc, :], P.pa[:, 82 + kc:83 + kc], rstd[:, :], ALU.mult, ALU.mult,
                    reads=[pk, P.pa, rstd], writes=[ckv])
            for h in range(NH):
                for (t0, n) in TBLK:
                    ps = kb.next_ps()
                    mm(kb, ps[:, 0:n], ps, [(wk[:, kc, h, :], ckv[:, kc, t0:t0 + n]) for kc in range(4)], reads=[wk, ckv])
                    evac_store(ps, n, A.kTn[h, :, t0:t0 + n])
            wv2 = wvv[:, :, :, :].rearrange("p k h d -> p k (h d)")
            for ti in range(18):
                for hb in range(2):
                    ps = kb.next_ps()
                    mm(kb, ps[:, :], ps, [(ckv[:, kc, ti * 128:(ti + 1) * 128], wv2[:, kc, hb * 512:(hb + 1) * 512]) for kc in range(4)],
                       reads=[wvv, ckv])
                    evac_store(ps, 512, A.vtm[ti * 128:(ti + 1) * 128, hb * 512:(hb + 1) * 512])
            memset(kb, kb.dve, kro[:, :], 0.0, writes=[kro])
            for (t0, n) in TBLK[:4]:
                asb, tmp1, tmp2 = rope_bufs()
                cp(kb, kb.act, asb[:, 0:n], kr[:, t0:t0 + n], reads=[kr], writes=[asb])
                rope_apply(kb, C, asb, n, t0, rcos, rsin, kro[0:64, t0:t0 + n], kro, tmp1, tmp2)
            cp(kb, kb.dve, kro[0:64, L:T], kr[:, L:T], reads=[kr], writes=[kro])
            kb.dma(kb.sp, [(A.krT[:, :], kro[:, :])], src=kro)
            kb.barrier()
        with ExitStack() as s2:
            pq = kb.sb(s2, "pq", [128, 6, T], BF16)
            qn = kb.sb(s2, "qn", [128, 6, T], BF16)
            wq = kb.sb(s2, "wq", [128, 6, 1536], BF16)
            qro = [kb.sb(s2, f"qro{i}", [128, 512], BF16) for i in range(2)]
            kb.dma(kb.sp, [(pq[:, :, :], A.pT[OFF_Q:OFF_Q + 768, :].rearrange("(kc p) t -> p kc t", p=128))], dst=pq)
            kb.dma(kb.pool, [(wq[:, :, :], A.w_uq[li].rearrange("(kc p) n -> p kc n", p=128))], dst=wq)
            fm_rstd(kb, C, pq, 6, T, 768, rstd, sq)
            for kc in range(6):
                stt(kb, kb.dve, qn[:, kc, :], pq[:, kc, :], P.pa[:, 76 + kc:77 + kc], rstd[:, :], ALU.mult, ALU.mult,
                    reads=[pq, P.pa, rstd], writes=[qn])
            for i in range(2):
                memset(kb, kb.dve, qro[i][:, :], 0.0, writes=[qro[i]])
            blks = TBLK[:4] if last else TBLK
            rc = [0]
            for h in range(NH):
                for (t0, n) in blks:
                    ps = kb.next_ps()
                    mm(kb, ps[:, 0:n], ps, [(wq[:, kc, h * 192:h * 192 + 128], qn[:, kc, t0:t0 + n]) for kc in range(6)], reads=[wq, qn])
                    evac_store(ps, n, A.qTn[h, :, t0:t0 + n])
                    ps = kb.next_ps()
                    mm(kb, ps[0:64, 0:n], ps, [(wq[:, kc, h * 192 + 128:h * 192 + 192], qn[:, kc, t0:t0 + n]) for kc in range(6)], reads=[wq, qn])
                    qo = qro[rc[0] % 2]
                    rc[0] += 1
                    if t0 < L:
                        asb, tmp1, tmp2 = rope_bufs()
                        cp(kb, kb.act, asb[:, 0:n], ps[0:64, 0:n], reads=[ps], writes=[asb])
                        rope_apply(kb, C, asb, n, t0, rcos, rsin, qo[0:64, 0:n], qo, tmp1, tmp2)
                    else:
                        cp(kb, kb.act, qo[0:64, 0:n], ps[0:64, 0:n], reads=[ps], writes=[qo])
                    kb.dma(kb.sp, [(A.qTr[h, :, t0:t0 + n], qo[:, 0:n])], src=qo)
            kb.barrier()
        kb.barrier()


def phase_attn(kb, A, C, P, li):
    last = li == DEPTH - 1
    with ExitStack() as st:
        krt = kb.sb(st, "krt", [128, T], BF16)
        kb.dma(kb.sp, [(krt[:, :], A.krT)], dst=krt)
        kts = [kb.sb(st, f"kt{i}", [128, T], BF16) for i in range(2)]
        qns = [kb.sb(st, f"qn{i}", [128, T], BF16) for i in range(2)]
        qrs = [kb.sb(st, f"qr{i}", [128, T], BF16) for i in range(2)]
        vhs = [kb.sb(st, f"vh{i}", [128, 18, 128], BF16) for i in range(2)]
        pts = [kb.sb(st, f"pt{i}", [128, 18, 512], BF16) for i in range(2)]
        recs = [kb.sb(st, f"rec{i}", [128, 512], F32) for i in range(2)]
        ots = [kb.sb(st, f"ot{i}", [128, 512], BF16) for i in range(2)]
        pc = [0]
        vsrc = A.vtm.rearrange("(t p) d -> p t d", p=128)

        def load_head(h):
            i = h % 2
            kb.dma(kb.sp, [(kts[i][:, :], A.kTn[h])], dst=kts[i])
            kb.dma(kb.sp, [(qns[i][:, :], A.qTn[h])], dst=qns[i])
            kb.dma(kb.sp, [(qrs[i][:, :], A.qTr[h])], dst=qrs[i])
            kb.dma(kb.sp, [(vhs[i][:, :, :], vsrc[:, :, h * 128:(h + 1) * 128])], dst=vhs[i])

        load_head(0)
        for h in range(NH):
            if h + 1 < NH:
                load_head(h + 1)
            i = h % 2
            kt, qn, qr, vh = kts[i], qns[i], qrs[i], vhs[i]
            jobs = [(t0, n, list(range(18))) for (t0, n) in TBLK[:4]]
            if not last:
                jobs.append((L, 256, [16, 17]))
            for (t0, n, chunks) in jobs:
                pt = pts[pc[0] % 2]
                ot = ots[pc[0] % 2]
                rec = recs[pc[0] % 2]
                pc[0] += 1
                for ci, ch in enumerate(chunks):
                    ps = kb.next_ps()
                    mm(kb, ps[:, 0:n], ps, [(kt[:, ch * 128:(ch + 1) * 128], qn[:, t0:t0 + n]),
                                            (krt[:, ch * 128:(ch + 1) * 128], qr[:, t0:t0 + n])], reads=[kt, qn, krt, qr])
                    act(kb, pt[:, ci, 0:n], ps[:, 0:n], AF.Exp, reads=[ps], writes=[pt], scale=SCALE)
                pso = kb.next_ps()
                mm(kb, pso[:, 0:n], pso, [(vh[:, ch, :], pt[:, ci, 0:n]) for ci, ch in enumerate(chunks)], reads=[vh, pt])
                pss = kb.next_ps()
                mm(kb, pss[:, 0:n], pss, [(C.onesb[:, :], pt[:, ci, 0:n]) for ci, ch in enumerate(chunks)], reads=[C.onesb, pt])
                kb.op(kb.dve, lambda e, n=n, pss=pss, rec=rec: e.reciprocal(out=rec[:, 0:n], in_=pss[:, 0:n]), reads=[pss], writes=[rec])
                tt(kb, kb.dve, ot[:, 0:n], pso[:, 0:n], rec[:, 0:n], ALU.mult, reads=[pso, rec], writes=[ot])
                kb.dma(kb.sp, [(A.olT[512 + h * 128:512 + (h + 1) * 128, t0:t0 + n], ot[:, 0:n])], src=ot)
        kb.barrier()


def phase_conv(kb, A, C, P, li):
    last = li == DEPTH - 1
    seqs = [(0, L)] if last else [(0, L), (L, LC)]
    with ExitStack() as st:
        yc = kb.sb(st, "yc", [128, 4, T], F32)
        zl = kb.sb(st, "zl", [128, T + 60], BF16)
        dg = kb.sb(st, "dg", [128, 31, 128], BF16)
        ab = [kb.sb(st, f"ab{i}", [128, T], BF16) for i in range(2)]
        sgm = kb.sb(st, "sgm", [128, T], BF16)
        memset(kb, kb.dve, zl[:, :], 0.0, writes=[zl])
        zoff = {0: 15, L: L + 30 + 15}
        for cc in range(4):
            kb.dma(kb.sp, [(ab[0][:, :], A.pT[cc * 128:(cc + 1) * 128, :])], dst=ab[0])
            kb.dma(kb.sp, [(ab[1][:, :], A.pT[512 + cc * 128:512 + (cc + 1) * 128, :])], dst=ab[1])
            act(kb, sgm[:, :], ab[1][:, :], AF.Sigmoid, reads=[ab[1]], writes=[sgm])
            for (o, n) in seqs:
                tt(kb, kb.dve, zl[:, zoff[o]:zoff[o] + n], ab[0][:, o:o + n], sgm[:, o:o + n], ALU.mult, reads=[ab[0], sgm], writes=[zl])
            for k in range(31):
                ts(kb, kb.dve, dg[:, k, :], C.identb[:, :], P.pb[:, k * 4 + cc:k * 4 + cc + 1], None, ALU.mult, None,
                   reads=[C.identb, P.pb], writes=[dg])
            for (o, n) in seqs:
                for b0 in range(0, n, 512):
                    nn = min(512, n - b0)
                    ps = kb.next_ps()
                    z0 = zoff[o] - 15 + b0
                    mm(kb, ps[:, 0:nn], ps, [(dg[:, k, :], zl[:, z0 + k:z0 + k + nn]) for k in range(31)], reads=[dg, zl])
                    ts(kb, kb.dve, yc[:, cc, o + b0:o + b0 + nn], ps[:, 0:nn], P.pa[:, 64 + cc:65 + cc], None, ALU.add, None,
                       reads=[ps, P.pa], writes=[yc])
        ysq = kb.sb(st, "ysq", [128, 4, 512], F32)
        mean = kb.sb(st, "mean", [128, 512], F32)
        msq = kb.sb(st, "msq", [128, 512], F32)
        rstd = kb.sb(st, "rstd", [128, 512], F32)
        t1 = kb.sb(st, "t1", [128, 512], F32)
        ots = [kb.sb(st, f"ot{i}", [128, 512], BF16) for i in range(2)]
        oc = 0
        for (t0, n) in (TBLK[:4] if last else TBLK):
            for cc in range(4):
                act(kb, ysq[:, cc, 0:n], yc[:, cc, t0:t0 + n], AF.Square, reads=[yc], writes=[ysq])
            psm = kb.next_ps()
            mm(kb, psm[:, 0:n], psm, [(C.ones32[:, :], yc[:, cc, t0:t0 + n]) for cc in range(4)], reads=[C.ones32, yc])
            psq = kb.next_ps()
            mm(kb, psq[:, 0:n], psq, [(C.ones32[:, :], ysq[:, cc, 0:n]) for cc in range(4)], reads=[C.ones32, ysq])
            act(kb, mean[:, 0:n], psm[:, 0:n], AF.Copy, reads=[psm], writes=[mean], scale=1.0 / 512)
            tt(kb, kb.dve, msq[:, 0:n], mean[:, 0:n], mean[:, 0:n], ALU.mult, reads=[mean], writes=[msq])
            stt(kb, kb.dve, rstd[:, 0:n], psq[:, 0:n], 1.0 / 512, msq[:, 0:n], ALU.mult, ALU.subtract, reads=[psq, msq], writes=[rstd])
            act(kb, rstd[:, 0:n], rstd[:, 0:n], AF.Sqrt, reads=[rstd], writes=[rstd], bias=EPS, scale=1.0)
            kb.op(kb.dve, lambda e, n=n: e.reciprocal(out=rstd[:, 0:n], in_=rstd[:, 0:n]), reads=[rstd], writes=[rstd])
            for cc in range(4):
                tt(kb, kb.dve, t1[:, 0:n], yc[:, cc, t0:t0 + n], mean[:, 0:n], ALU.subtract, reads=[yc, mean], writes=[t1])
                tt(kb, kb.dve, t1[:, 0:n], t1[:, 0:n], rstd[:, 0:n], ALU.mult, reads=[t1, rstd], writes=[t1])
                ot = ots[oc % 2]
                oc += 1
                act(kb, ot[:, 0:n], t1[:, 0:n], AF.Silu, reads=[t1, P.pa], writes=[ot], bias=P.pa[:, 72 + cc:73 + cc],
                    scale=P.pa[:, 68 + cc:69 + cc])
                kb.dma(kb.sp, [(A.olT[cc * 128:(cc + 1) * 128, t0:t0 + n], ot[:, 0:n])], src=ot)
        kb.barrier()


def phase_hyena(kb, A, C, P, li, off, Ls, tag):
    nT = Ls // 128
    blks = [(b0, min(512, Ls - b0)) for b0 in range(0, Ls, 512)]
    iblk = [(b0, min(256, Ls - b0)) for b0 in range(0, Ls, 256)]
    dC = getattr(C.d, "dftc_" + tag)
    dS = getattr(C.d, "dfts_" + tag)
    inv_scale = 2.0 / (2 * Ls)
    with ExitStack() as st:
        pz = [kb.sb(st, f"pz{i}", [128, Ls + 2], BF16) for i in range(2)]
        uo = [kb.sb(st, f"uo{i}", [128, Ls], BF16) for i in range(2)]
        for i in range(2):
            memset(kb, kb.dve, pz[i][:, :], 0.0, writes=[pz[i]])
        for j in range(12):
            p_, u_ = pz[j % 2], uo[j % 2]
            kb.dma(kb.sp, [(p_[:, 1:Ls + 1], A.pT[OFF_HY + j * 128:OFF_HY + (j + 1) * 128, off:off + Ls])], dst=p_)
            ts(kb, kb.dve, u_[:, :], p_[:, 1:Ls + 1], P.pd[:, 12 + j:13 + j], P.pd[:, 36 + j:37 + j], ALU.mult, ALU.add,
               reads=[p_, P.pd], writes=[u_])
            stt(kb, kb.dve, u_[:, :], p_[:, 0:Ls], P.pd[:, j:j + 1], u_[:, :], ALU.mult, ALU.add, reads=[p_, P.pd, u_], writes=[u_])
            stt(kb, kb.dve, u_[:, :], p_[:, 2:Ls + 2], P.pd[:, 24 + j:25 + j], u_[:, :], ALU.mult, ALU.add, reads=[p_, P.pd, u_], writes=[u_])
            kb.dma(kb.sp, [(A.uTd[j * 128:(j + 1) * 128, off:off + Ls], u_[:, :])], src=u_)
        kb.barrier()
    with ExitStack() as st:
        h1T = kb.sb(st, "h1T", [65, Ls], F32)
        h2T = kb.sb(st, "h2T", [65, Ls], F32)
        zT = kb.sb(st, "zT", [33, Ls], F32)
        w1 = kb.sb(st, "w1", [33, 64], F32)
        w2 = kb.sb(st, "w2", [64, 64], F32)
        w3 = kb.sb(st, "w3", [65, 2048], F32)
        sc = kb.sb(st, "sc", [64, 4], F32)
        negpi = kb.sb(st, "negpi", [128, 1], F32)
        hb = kb.sb(st, "hb", [1, 2, 512], F32)
        alt = kb.sb(st, "alt", [128, 1], BF16)
        altr = kb.sb(st, "altr", [1, Ls], BF16)
        tmpa = kb.sb(st, "tmpa", [64, 512], F32)
        tmpb = kb.sb(st, "tmpb", [64, 512], F32)
        tmpi = kb.sb(st, "tmpi", [64, 512], mybir.dt.int32)
        kb.dma(kb.sp, [(zT[:, :], getattr(C.d, "hz_" + tag))], dst=zT)
        kb.dma(kb.sp, [(w1[:, :], A.hy_w1[li])], dst=w1)
        kb.dma(kb.sp, [(w2[:, :], A.hy_w2[li])], dst=w2)
        kb.dma(kb.sp, [(w3[0:64, :], A.hy_w3[li]), (w3[64:65, :], A.hy_b3[li])], dst=w3)
        kb.dma(kb.sp, [(sc[:, 0:1], A.hy_b1[li]), (sc[:, 1:2], A.hy_freq1[li]), (sc[:, 2:3], A.hy_b2[li]), (sc[:, 3:4], A.hy_freq2[li])], dst=sc)
        kb.dma(kb.sp, [(hb[:, :, :], A.hy_bias[li:li + 1])], dst=hb)
        kb.dma(kb.sp, [(alt[:, :], getattr(C.d, "alt_" + tag)[0:128, :])], dst=alt)
        kb.dma(kb.sp, [(altr[:, :], getattr(C.d, "altr_" + tag))], dst=altr)
        memset(kb, kb.dve, negpi[:, :], -PI, writes=[negpi])
        memset(kb, kb.dve, h2T[64:65, :], 1.0, writes=[h2T])

        def sin_layer(dst, wt, kdim, src, bcol, fcol):
            for (b0, n) in blks:
                ps = kb.next_ps()
                mm(kb, ps[0:64, 0:n], ps, [(wt[0:kdim, :], src[0:kdim, b0:b0 + n])], reads=[wt, src])
                ts(kb, kb.dve, tmpa[:, 0:n], ps[0:64, 0:n], sc[:, bcol:bcol + 1], sc[:, fcol:fcol + 1], ALU.add, ALU.mult,
                   reads=[ps, sc], writes=[tmpa])
                ts(kb, kb.dve, tmpa[:, 0:n], tmpa[:, 0:n], 1.0 / (2 * PI), None, ALU.mult, None, reads=[tmpa], writes=[tmpa])
                cp(kb, kb.dve, tmpi[:, 0:n], tmpa[:, 0:n], reads=[tmpa], writes=[tmpi])
                cp(kb, kb.dve, tmpb[:, 0:n], tmpi[:, 0:n], reads=[tmpi], writes=[tmpb])
                tt(kb, kb.dve, tmpa[:, 0:n], tmpa[:, 0:n], tmpb[:, 0:n], ALU.subtract, reads=[tmpa, tmpb], writes=[tmpa])
                ts(kb, kb.dve, tmpb[:, 0:n], tmpa[:, 0:n], 0.5, None, ALU.is_gt, None, reads=[tmpa], writes=[tmpb])
                tt(kb, kb.dve, tmpa[:, 0:n], tmpa[:, 0:n], tmpb[:, 0:n], ALU.subtract, reads=[tmpa, tmpb], writes=[tmpa])
                ts(kb, kb.dve, tmpb[:, 0:n], tmpa[:, 0:n], -0.5, None, ALU.is_lt, None, reads=[tmpa], writes=[tmpb])
                tt(kb, kb.dve, tmpa[:, 0:n], tmpa[:, 0:n], tmpb[:, 0:n], ALU.add, reads=[tmpa, tmpb], writes=[tmpa])
                act(kb, dst[0:64, b0:b0 + n], tmpa[:, 0:n], AF.Sin, reads=[tmpa], writes=[dst], scale=2 * PI)

        sin_layer(h1T, w1, 33, zT, 0, 1)
        sin_layer(h2T, w2, 64, h1T, 2, 3)

        hk = kb.sb(st, "hk", [128, nT, 2, 512], BF16)
        utm = kb.sb(st, "utm", [128, nT, 512], BF16)
        yre = kb.sb(st, "yre", [128, nT, 512], BF16)
        yim = kb.sb(st, "yim", [128, nT, 512], BF16)
        yny = kb.sb(st, "yny", [1, 512], BF16)
        nyf = kb.sb(st, "nyf", [1, 2, 512], F32)
        zTt = kb.sb(st, "zTt", [128, 4, Ls], BF16)
        for o in range(2):
            with ExitStack() as s2:
                wins = [kb.sb(s2, f"win{i}", [128, 512], F32) for i in range(2)]
                hf = [kb.sb(s2, f"hf{i}", [128, 512], F32) for i in range(2)]
                wsrc = getattr(C.d, "hwin_" + tag)
                for pt in range(nT):
                    win = wins[pt % 2]
                    kb.dma(kb.sp, [(win[:, :], wsrc[pt * 128:(pt + 1) * 128, :])], dst=win)
                    for d in range(2):
                        ps = kb.next_ps()
                        cb = (o * 2 + d) * 512
                        mm(kb, ps[:, :], ps, [(h2T[0:65, pt * 128:(pt + 1) * 128], w3[0:65, cb:cb + 512])], reads=[h2T, w3])
                        tt(kb, kb.dve, hf[d][:, :], ps[:, :], win[:, :], ALU.mult, reads=[ps, win], writes=[hf[d]])
                    if pt == 0:
                        tt(kb, kb.dve, hf[0][0:1, :], hf[0][0:1, :], hb[0:1, o, :], ALU.add, reads=[hf[0], hb], writes=[hf[0]])
                        memset(kb, kb.dve, hf[1][0:1, :], 0.0, writes=[hf[1]])
                    tt(kb, kb.dve, hk[:, pt, 0, :], hf[0][:, :], hf[1][:, :], ALU.add, reads=hf, writes=[hk])
                    tt(kb, kb.dve, hk[:, pt, 1, :], hf[1][:, :], hf[0][:, :], ALU.subtract, reads=hf, writes=[hk])
                kb.barrier()
            with ExitStack() as s2:
                srcs = [kb.sb(s2, f"src{i}", [128, Ls], BF16) for i in range(2)]
                for cc in range(4):
                    sr = srcs[cc % 2]
                    if o == 0:
                        kb.dma(kb.sp, [(sr[:, :], A.uTd[cc * 128:(cc + 1) * 128, off:off + Ls])], dst=sr)
                        rd = sr
                        rap = sr
                    else:
                        rd = zTt
                    for g in range(0, nT, 8):
                        ph = kb.psb[(g // 8) % 2]
                        ng = min(8, nT - g)
                        for k8 in range(ng):
                            pt = g + k8
                            src_ap = sr[:, pt * 128:(pt + 1) * 128] if o == 0 else zTt[:, cc, pt * 128:(pt + 1) * 128]
                            tr(kb, ph[:, k8 * 128:(k8 + 1) * 128], ph, src_ap, C.identb[:, :], reads=[rd, C.identb])
                        for k8 in range(ng):
                            pt = g + k8
                            cp(kb, kb.act if k8 % 2 else kb.dve, utm[:, pt, cc * 128:(cc + 1) * 128], ph[:, k8 * 128:(k8 + 1) * 128],
                               reads=[ph], writes=[utm])
                kb.barrier()
            with ExitStack() as s2:
                ring = [kb.sb(s2, f"cs{i}", [128, nT, 2, 128], BF16) for i in range(2)]
                psb_ = [kb.sb(s2, f"pq{i}", [128, 512], F32) for i in range(2)]
                t_ = [kb.sb(s2, f"tq{i}", [128, 512], F32) for i in range(2)]
                cv = dC.rearrange("(sc p) f -> p sc f", p=128)
                sv = dS.rearrange("(sc p) f -> p sc f", p=128)
                loads = [(lambda t, fc=fc: [(t[:, :, 0, :], cv[:, :, fc * 128:(fc + 1) * 128]), (t[:, :, 1, :], sv[:, :, fc * 128:(fc + 1) * 128])])
                         for fc in range(nT)]

                def compf(fc, slot):
                    pA, pB, pP, pQ = kb.next_ps(), kb.next_ps(), kb.next_ps(), kb.next_ps()
                    mm(kb, pP[:, :], pP, [(slot[:, s_, 0, :], hk[:, s_, 0, :]) for s_ in range(nT)], reads=[slot, hk])
                    mm(kb, pQ[:, :], pQ, [(slot[:, s_, 1, :], hk[:, s_, 1, :]) for s_ in range(nT)], reads=[slot, hk])
                    mm(kb, pA[:, :], pA, [(slot[:, s_, 0, :], utm[:, s_, :]) for s_ in range(nT)], reads=[slot, utm])
                    mm(kb, pB[:, :], pB, [(slot[:, s_, 1, :], utm[:, s_, :]) for s_ in range(nT)], reads=[slot, utm])
                    cp(kb, kb.act, psb_[0][:, :], pP[:, :], reads=[pP], writes=[psb_[0]])
                    cp(kb, kb.act, psb_[1][:, :], pQ[:, :], reads=[pQ], writes=[psb_[1]])
                    tt(kb, kb.dve, t_[0][:, :], pA[:, :], psb_[0][:, :], ALU.mult, reads=[pA, psb_[0]], writes=[t_[0]])
                    tt(kb, kb.dve, t_[1][:, :], pB[:, :], psb_[1][:, :], ALU.mult, reads=[pB, psb_[1]], writes=[t_[1]])
                    tt(kb, kb.dve, yre[:, fc, :], t_[0][:, :], t_[1][:, :], ALU.add, reads=t_, writes=[yre])
                    tt(kb, kb.dve, t_[0][:, :], pB[:, :], psb_[0][:, :], ALU.mult, reads=[pB, psb_[0]], writes=[t_[0]])
                    tt(kb, kb.dve, t_[1][:, :], pA[:, :], psb_[1][:, :], ALU.mult, reads=[pA, psb_[1]], writes=[t_[1]])
                    tt(kb, kb.dve, yim[:, fc, :], t_[0][:, :], t_[1][:, :], ALU.subtract, reads=t_, writes=[yim])

                stream(kb, ring, loads, compf, kb.sp)
                pn = kb.next_ps()
                mm(kb, pn[0:1, :], pn, [(alt[:, 0:1], utm[:, s_, :]) for s_ in range(nT)], reads=[alt, utm])
                cp(kb, kb.dve, nyf[:, 0, :], pn[0:1, :], reads=[pn], writes=[nyf])
                pn2 = kb.next_ps()
                mm(kb, pn2[0:1, :], pn2, [(alt[:, 0:1], hk[:, s_, 0, :]) for s_ in range(nT)], reads=[alt, hk])
                tt(kb, kb.dve, yny[:, :], pn2[0:1, :], nyf[:, 0, :], ALU.mult, reads=[pn2, nyf], writes=[yny])
                ts(kb, kb.dve, yre[0:1, 0, :], yre[0:1, 0, :], 0.5, None, ALU.mult, None, reads=[yre], writes=[yre])
                kb.barrier()
            with ExitStack() as s2:
                ring = [kb.sb(s2, f"ci{i}", [128, nT, 2, 256], BF16) for i in range(2)]
                gts = [kb.sb(s2, f"gt{i}", [128, 256], BF16) for i in range(2)]
                ots = [kb.sb(s2, f"ot{i}", [128, 256], BF16) for i in range(2)]
                r32 = kb.sb(s2, "r32", [128, 256], F32)
                cv = dC.rearrange("(fc p) t -> p fc t", p=128)
                sv = dS.rearrange("(fc p) t -> p fc t", p=128)
                loads = [(lambda t, b0=b0, n=n: [(t[:, :, 0, 0:n], cv[:, :, b0:b0 + n]), (t[:, :, 1, 0:n], sv[:, :, b0:b0 + n])])
                         for (b0, n) in iblk]
                gc = [0]

                def compi(j, slot):
                    b0, n = iblk[j]
                    for cc in range(4):
                        ps = kb.next_ps()
                        terms = [(yre[:, fc, cc * 128:(cc + 1) * 128], slot[:, fc, 0, 0:n]) for fc in range(nT)]
                        terms += [(yim[:, fc, cc * 128:(cc + 1) * 128], slot[:, fc, 1, 0:n]) for fc in range(nT)]
                        terms += [(yny[0:1, cc * 128:(cc + 1) * 128], altr[0:1, b0:b0 + n])]
                        mm(kb, ps[:, 0:n], ps, terms, reads=[yre, yim, yny, altr, slot])
                        gt = gts[gc[0] % 2]
                        ot = ots[gc[0] % 2]
                        gc[0] += 1
                        grow_ = (4 if o == 0 else 8) + cc
                        kb.dma(kb.sp, [(gt[:, 0:n], A.uTd[grow_ * 128:(grow_ + 1) * 128, off + b0:off + b0 + n])], dst=gt)
                        if o == 0:
                            stt(kb, kb.dve, zTt[:, cc, b0:b0 + n], ps[:, 0:n], inv_scale, gt[:, 0:n], ALU.mult, ALU.mult,
                                reads=[ps, gt], writes=[zTt])
                        else:
                            stt(kb, kb.dve, ot[:, 0:n], ps[:, 0:n], inv_scale, gt[:, 0:n], ALU.mult, ALU.mult, reads=[ps, gt], writes=[ot])
                            kb.dma(kb.sp, [(A.olT[1536 + cc * 128:1536 + (cc + 1) * 128, off + b0:off + b0 + n], ot[:, 0:n])], src=ot)

                stream(kb, ring, loads, compi, kb.sp)
                kb.barrier()
        kb.barrier()
```

```python
import math
from contextlib import ExitStack
import numpy as np
import ml_dtypes
import concourse.bass as bass
import concourse.mybir as mybir
from concourse.bass_utils import run_bass_kernel_spmd

F32 = mybir.dt.float32
BF16 = mybir.dt.bfloat16
AF = mybir.ActivationFunctionType
ALU = mybir.AluOpType
AX = mybir.AxisListType

D = 2048
L = 2048
LC = 256
T = L + LC
DEPTH = 2
N_IN = 3904
OFF_Q, OFF_KV, OFF_HY = 1024, 1792, 2368
NH = 8
EPS = 1e-6
NE = 16
DFF = 1024
SCALE = 192 ** -0.5
PI = math.pi


class Tile:
    __slots__ = ("ap", "w", "r", "dsem", "name", "excl")

    def __init__(self, ap, name="", excl=False):
        self.excl = excl
        self.ap = ap
        self.w = None
        self.r = []
        self.dsem = None
        self.name = name

    def __getitem__(self, idx):
        return self.ap[idx]


class Eng:
    def __init__(self, e, sem, name):
        self.e = e
        self.sem = sem
        self.cnt = 0
        self.seen = {}
        self.name = name

    def wait(self, tok):
        if tok is None:
            return
        sem, val = tok
        if self.name == "pe" and sem is self.sem:
            return
        if self.seen.get(sem, 0) >= val:
            return
        self.e.wait_ge(sem, val)
        self.seen[sem] = val


class KB:
    def __init__(self, nc, es, nsem=80):
        self.nc = nc
        self.es = es
        mk = lambda n: es.enter_context(nc.semaphore(n))
        self.pe = Eng(nc.tensor, mk("s_pe"), "pe")
        self.act = Eng(nc.scalar, mk("s_act"), "act")
        self.dve = Eng(nc.vector, mk("s_dve"), "dve")
        self.pool = Eng(nc.gpsimd, mk("s_pool"), "pool")
        self.sp = Eng(nc.sync, mk("s_sp"), "sp")
        self.engs = [self.pe, self.act, self.dve, self.pool, self.sp]
        self.free_sems = [mk(f"s_d{i}") for i in range(nsem)]
        self.semval = {}
        self.pending = []
        self.phase_tiles = []
        self.pes = None
        self.n_inst = 0

    def sb(self, stack, name, shape, dt):
        self.uid = getattr(self, "uid", 0) + 1
        name = f"{name}_{self.uid}"
        t = Tile(stack.enter_context(self.nc.sbuf_tensor(name, list(shape), dt)), name)
        self.phase_tiles.append(t)
        return t

    def sub(self, ap, name=""):
        t = Tile(ap, name)
        self.phase_tiles.append(t)
        return t

    def _dsem(self, t):
        if t.dsem is None:
            assert self.free_sems, "out of DMA semaphores"
            t.dsem = self.free_sems.pop()
        return t.dsem

    def op(self, eng, fn, reads=(), writes=()):
        for t in reads:
            eng.wait(t.w)
            if t.excl:
                for r in t.r:
                    if r[0] is not eng.sem:
                        eng.wait(r)
        for t in writes:
            eng.wait(t.w)
            for r in t.r:
                eng.wait(r)
        ins = fn(eng.e)
        eng.cnt += 1
        ins.then_inc(eng.sem, 1)
        tok = (eng.sem, eng.cnt)
        for t in reads:
            t.r.append(tok)
        for t in writes:
            t.w = tok
            t.r = []
        self.n_inst += 1
        return tok

    def group(self, eng, fns, reads=(), writes=()):
        for t in reads:
            eng.wait(t.w)
            if t.excl:
                for r in t.r:
                    if r[0] is not eng.sem:
                        eng.wait(r)
        for t in writes:
            eng.wait(t.w)
            for r in t.r:
                eng.wait(r)
        ins = None
        for fn in fns:
            ins = fn(eng.e)
        eng.cnt += 1
        ins.then_inc(eng.sem, 1)
        tok = (eng.sem, eng.cnt)
        for t in reads:
            t.r.append(tok)
        for t in writes:
            t.w = tok
            t.r = []
        self.n_inst += len(fns)
        return tok

    def dma(self, q, pairs, dst=None, src=None, **kw):
        if src is not None:
            q.wait(src.w)
        if dst is not None:
            q.wait(dst.w)
            for r in dst.r:
                q.wait(r)
        t = dst if dst is not None else src
        sem = self._dsem(t)
        v = self.semval.get(sem, 0)
        for (o, i) in pairs:
            q.e.dma_start(out=o, in_=i, **kw).then_inc(sem, 16)
            v += 16
        self.semval[sem] = v
        tok = (sem, v)
        if dst is not None:
            dst.w = tok
            dst.r = []
            if src is not None:
                src.r.append(tok)
        else:
            src.r.append(tok)
        self.pending.append(tok)
        self.n_inst += len(pairs)
        return tok

    def barrier(self):
        toks = [(e.sem, e.cnt) for e in self.engs if e.cnt > 0] + self.pending
        best = {}
        for s, v in toks:
            if best.get(s, 0) < v:
                best[s] = v
        for e in self.engs:
            for s, v in best.items():
                if s is e.sem:
                    continue
                e.wait((s, v))
        self.pending = []
        for t in self.phase_tiles:
            if t.dsem is not None:
                self.free_sems.append(t.dsem)
                t.dsem = None
            t.w = None
            t.r = []
        self.phase_tiles = []

    def init_psum(self, stack):
        self.ps = [Tile(stack.enter_context(self.nc.psum_tensor(f"ps{i}", [128, 512], F32)), f"ps{i}", True) for i in range(6)]
        self.psb = [Tile(stack.enter_context(self.nc.psum_tensor(f"psb{i}", [128, 1024], BF16)), f"psb{i}", True) for i in range(2)]
        self.ps_i = 0

    def next_ps(self):
        t = self.ps[self.ps_i % len(self.ps)]
        self.ps_i += 1
        return t


def mm(kb, ps_ap, ps_tile, terms, reads):
    n = len(terms)
    fns = []
    for i, (l, r) in enumerate(terms):
        fns.append(lambda e, l=l, r=r, i=i: e.matmul(ps_ap, lhsT=l, rhs=r, start=(i == 0), stop=(i == n - 1)))
    return kb.group(kb.pe, fns, reads=reads, writes=[ps_tile])


def tr(kb, ps_ap, ps_tile, in_ap, ident_ap, reads):
    return kb.op(kb.pe, lambda e: e.transpose(ps_ap, in_ap, ident_ap), reads=reads, writes=[ps_tile])


def act(kb, out, in_, func, reads, writes, bias=0.0, scale=1.0, accum_out=None):
    kw = {}
    if accum_out is not None:
        kw["accum_out"] = accum_out
    return kb.op(kb.act, lambda e: e.activation(out=out, in_=in_, func=func, bias=bias, scale=scale, **kw),
                 reads=reads, writes=writes)


def ts(kb, eng, out, in0, s1, s2, op0, op1, reads, writes):
    if s2 is None:
        return kb.op(eng, lambda e: e.tensor_scalar(out=out, in0=in0, scalar1=s1, scalar2=None, op0=op0),
                     reads=reads, writes=writes)
    return kb.op(eng, lambda e: e.tensor_scalar(out=out, in0=in0, scalar1=s1, scalar2=s2, op0=op0, op1=op1),
                 reads=reads, writes=writes)


def tt(kb, eng, out, in0, in1, op, reads, writes):
    return kb.op(eng, lambda e: e.tensor_tensor(out=out, in0=in0, in1=in1, op=op), reads=reads, writes=writes)


def stt(kb, eng, out, in0, scalar, in1, op0, op1, reads, writes):
    return kb.op(eng, lambda e: e.scalar_tensor_tensor(out=out, in0=in0, scalar=scalar, in1=in1, op0=op0, op1=op1),
                 reads=reads, writes=writes)


def cp(kb, eng, out, in_, reads, writes):
    if eng is kb.act:
        return kb.op(eng, lambda e: e.copy(out=out, in_=in_), reads=reads, writes=writes)
    return kb.op(eng, lambda e: e.tensor_copy(out=out, in_=in_), reads=reads, writes=writes)


def memset(kb, eng, ap, val, writes):
    return kb.op(eng, lambda e: e.memset(ap, val), reads=(), writes=writes)


def bf(a):
    return np.ascontiguousarray(a).astype(ml_dtypes.bfloat16)


def make_consts():
    c = {}
    c["ident32"] = np.eye(128, dtype=np.float32)
    c["identb"] = bf(np.eye(128, dtype=np.float32))
    c["ones32"] = np.ones((128, 128), np.float32)
    c["onesb"] = bf(np.ones((128, 128), np.float32))
    pos = np.arange(L)
    r = (pos // 64).astype(np.float32)
    col = (pos % 64).astype(np.float32)
    inv = (10000.0 ** (-np.arange(16, dtype=np.float32) / 16)).astype(np.float32)
    ang_r = (r[None, :] * inv[:, None]).astype(np.float32)
    ang_c = (col[None, :] * inv[:, None]).astype(np.float32)
    cos = np.concatenate([np.cos(ang_r), np.cos(ang_r), np.cos(ang_c), np.cos(ang_c)], 0)
    sin = np.concatenate([-np.sin(ang_r), np.sin(ang_r), -np.sin(ang_c), np.sin(ang_c)], 0)
    c["rcos"] = cos.astype(np.float32)
    c["rsin"] = sin.astype(np.float32)
    perm = np.zeros((64, 64), np.float32)
    for m in range(64):
        k = m + 16 if (m % 32) < 16 else m - 16
        perm[k, m] = 1.0
    c["perm"] = bf(perm)
    for Ls, tag in ((L, "l"), (LC, "c")):
        t = np.arange(Ls, dtype=np.float32)
        tn = t / max(Ls - 1, 1)
        bands = np.linspace(1e-4, 15, 16, dtype=np.float32)
        ang = (np.float32(2.0 * math.pi / Ls) * t[:, None] * bands[None, :]).astype(np.float32)
        z = np.concatenate([tn[:, None], np.cos(ang), -np.sin(ang)], -1).astype(np.float32)
        c["hz_" + tag] = np.ascontiguousarray(z.T)
        deltas = np.abs(np.linspace(math.log(1e-2) / 1.5, math.log(1e-2) / 0.3, 512, dtype=np.float32))
        c["hwin_" + tag] = np.exp(-tn[:, None] * deltas[None, :]).astype(np.float32)
        n2 = 2 * Ls
        idx = (np.arange(Ls)[:, None].astype(np.int64) * np.arange(Ls)[None, :].astype(np.int64)) % n2
        a = idx.astype(np.float64) * (2.0 * math.pi / n2)
        c["dftc_" + tag] = bf(np.cos(a))
        c["dfts_" + tag] = bf(np.sin(a))
        alt = np.where(np.arange(Ls) % 2 == 0, 1.0, -1.0).astype(np.float32)
        c["alt_" + tag] = bf(alt[:, None] * np.ones((1, 1), np.float32))
        c["altr_" + tag] = bf(0.5 * alt[None, :])
    return c


def stream(kb, tiles, loads, compute, q, keep=0, **kw):
    n = len(loads)
    R = len(tiles)
    issued = 0
    for j in range(n):
        while issued < min(n, j + R - keep):
            t = tiles[issued % R]
            kb.dma(q, loads[issued](t), dst=t, **kw)
            issued += 1
        compute(j, tiles[j % R])


class NS:
    pass


def phase_params(kb, A, C, li, lst):
    P = NS()
    P.pa = kb.sb(lst, f"pa{li}", [128, 86], F32)
    P.pb = kb.sb(lst, f"pb{li}", [128, 124], F32)
    P.pc = kb.sb(lst, f"pc{li}", [128, 96], F32)
    P.pd = kb.sb(lst, f"pd{li}", [128, 48], F32)
    P.mod = kb.sb(lst, f"mod{li}", [128, 96, 2], F32)
    P.g1m = kb.sb(lst, f"g1m{li}", [128, 16, 2], F32)
    P.g2m = kb.sb(lst, f"g2m{li}", [128, 16, 2], F32)
    with ExitStack() as st:
        stA = kb.sb(st, "stA", [128, 128], F32)
        stB = kb.sb(st, "stB", [128, 128], F32)
        stC = kb.sb(st, "stC", [128, 128], F32)
        stD = kb.sb(st, "stD", [128, 128], F32)
        scT = kb.sb(st, "scT", [128, 2, 16], BF16)
        brow = kb.sb(st, "brow", [2, 4096], F32)
        grow = [kb.sb(st, f"grow{i}", [2, 512], F32) for i in range(2)]
        ring = [kb.sb(st, f"wr{i}", [128, 16, 512], BF16) for i in range(3)]
        q = kb.sp
        rowsA = [(A.c, 16), (A.c_ctx, 16), (A.norm1_g[li], 16), (A.norm2_g[li], 16), (A.conv_dw_b[li], 4),
                 (A.conv_ln_g[li], 4), (A.conv_ln_b[li], 4), (A.q_norm_g[li], 6), (A.kv_norm_g[li], 4)]
        r0 = 0
        pairs = []
        for ap, n in rowsA:
            pairs.append((stA[r0:r0 + n, :], ap))
            r0 += n
        assert r0 == 86
        kb.dma(q, pairs, dst=stA)
        kb.dma(q, [(stB[0:124, :], A.conv_dw_w[li])], dst=stB)
        kb.dma(q, [(stC[0:96, :], A.b_ada[li])], dst=stC)
        kb.dma(q, [(stD[0:36, :], A.hy_short_w[li]), (stD[36:48, :], A.hy_short_b[li])], dst=stD)
        bsrc = A.b_ada_row[li]
        kb.dma(q, [(brow[:, 0:2048], bsrc[:, 4096:6144].partition_broadcast(2)),
                   (brow[:, 2048:4096], bsrc[:, 10240:12288].partition_broadcast(2))], dst=brow)
        for stg, n, dstt in ((stA, 86, P.pa), (stB, 124, P.pb), (stC, 96, P.pc), (stD, 48, P.pd)):
            ps = kb.next_ps()
            tr(kb, ps[:, 0:n], ps, stg[0:n, :], C.ident32[0:n, 0:n], reads=[stg, C.ident32])
            cp(kb, kb.dve, dstt[:, :], ps[:, 0:n], reads=[ps], writes=[dstt])
        act(kb, scT[:, :, :], P.pa[:, 0:32].rearrange("p (s k) -> p s k", s=2), AF.Silu, reads=[P.pa], writes=[scT])

        wv = A.w_ada[li].rearrange("(kc p) n -> p kc n", p=128)
        loads = [(lambda t, j=j: [(t[:, :, :], wv[:, :, j * 512:(j + 1) * 512])]) for j in range(24)]

        def comp(j, slot):
            ps = kb.next_ps()
            for mc in range(4):
                terms = [(slot[:, kc, mc * 128:(mc + 1) * 128], scT[:, :, kc]) for kc in range(16)]
                mm(kb, ps[:, 2 * mc:2 * mc + 2], ps, terms, reads=[slot, scT])
            for mc in range(4):
                jj = j * 4 + mc
                ts(kb, kb.dve, P.mod[:, jj, :], ps[:, 2 * mc:2 * mc + 2], P.pc[:, jj:jj + 1], None, ALU.add, None,
                   reads=[ps, P.pc], writes=[P.mod])
            gsel = {8: 0, 9: 0, 10: 0, 11: 0, 20: 1, 21: 1, 22: 1, 23: 1}
            if j in gsel:
                g = gsel[j]
                blk = j % 4
                ps2 = kb.next_ps()
                terms = [(scT[:, :, kc], slot[:, kc, :]) for kc in range(16)]
                mm(kb, ps2[0:2, :], ps2, terms, reads=[slot, scT])
                gr = grow[j % 2]
                tt(kb, kb.dve, gr[:, :], ps2[0:2, :], brow[:, g * 2048 + blk * 512: g * 2048 + (blk + 1) * 512], ALU.add,
                   reads=[ps2, brow], writes=[gr])
                kb.dma(kb.sp, [(A.gates[li, g, :, blk * 512:(blk + 1) * 512], gr[:, :])], src=gr)

        stream(kb, ring, loads, comp, kb.pool)
        for s in range(2):
            stt(kb, kb.dve, P.g1m[:, :, s], P.mod[:, 16:32, s], 1.0, P.pa[:, 32:48], ALU.add, ALU.mult,
                reads=[P.mod, P.pa], writes=[P.g1m])
            stt(kb, kb.dve, P.g2m[:, :, s], P.mod[:, 64:80, s], 1.0, P.pa[:, 48:64], ALU.add, ALU.mult,
                reads=[P.mod, P.pa], writes=[P.g2m])
        kb.barrier()
    return P


def tok_rstd(kb, xt, junk, ss, rs, width):
    memset(kb, kb.dve, ss[:, 0:1], 0.0, writes=[ss])
    act(kb, junk[:, 0:width], xt[:, 0:width], AF.Square, reads=[xt, ss], writes=[junk, ss], accum_out=ss[:, 0:1])
    act(kb, rs[:, 0:1], ss[:, 0:1], AF.Sqrt, reads=[ss], writes=[rs], bias=EPS, scale=1.0 / width)
    kb.op(kb.dve, lambda e: e.reciprocal(out=rs[:, 0:1], in_=rs[:, 0:1]), reads=[rs], writes=[rs])


def phase_inproj(kb, A, C, P, li, xsrc, xcsrc):
    last = li == DEPTH - 1
    with ExitStack() as st:
        hT = kb.sb(st, "hT", [128, 16, T], BF16)
        hTt = [kb.sub(hT.ap[:, :, i * 128:(i + 1) * 128], f"hT{i}") for i in range(18)]
        xts = [kb.sb(st, f"xt{i}", [128, D], F32) for i in range(2)]
        xns = [kb.sb(st, f"xn{i}", [128, D], BF16) for i in range(2)]
        junk = kb.sb(st, "junk", [128, D], BF16)
        sss = [kb.sb(st, f"ss{i}", [128, 1], F32) for i in range(2)]
        rss = [kb.sb(st, f"rs{i}", [128, 1], F32) for i in range(2)]
        ring = [kb.sb(st, f"wr{i}", [128, 16, 512], BF16) for i in range(3)]
        ots = [kb.sb(st, f"ot{i}", [128, 512], BF16) for i in range(4)]

        def src_rows(ti):
            return xsrc[ti * 128:(ti + 1) * 128, :] if ti < 16 else xcsrc[(ti - 16) * 128:(ti - 15) * 128, :]

        loads = [(lambda t, ti=ti: [(t[:, :], src_rows(ti))]) for ti in range(18)]
        cnt = [0]

        def comp(ti, xt):
            s = 0 if ti < 16 else 1
            xn, ss, rs = xns[ti % 2], sss[ti % 2], rss[ti % 2]
            tok_rstd(kb, xt, junk, ss, rs, D)
            ts(kb, kb.dve, xn[:, :], xt[:, :], rs[:, 0:1], None, ALU.mult, None, reads=[xt, rs], writes=[xn])
            for g in range(2):
                ph = kb.psb[g]
                for k8 in range(8):
                    kc = g * 8 + k8
                    tr(kb, ph[:, k8 * 128:(k8 + 1) * 128], ph, xn[:, kc * 128:(kc + 1) * 128], C.identb[:, :],
                       reads=[xn, C.identb])
                for k8 in range(8):
                    kc = g * 8 + k8
                    o = hTt[ti][:, kc, :]
                    i_ = ph[:, k8 * 128:(k8 + 1) * 128]
                    if g == 0:
                        ts(kb, kb.dve, o, i_, P.g1m[:, kc, s:s + 1], P.mod[:, kc, s:s + 1], ALU.mult, ALU.add,
                           reads=[ph, P.g1m, P.mod], writes=[hTt[ti]])
                    else:
                        act(kb, o, i_, AF.Identity, reads=[ph, P.g1m, P.mod], writes=[hTt[ti]],
                            bias=P.mod[:, kc, s:s + 1], scale=P.g1m[:, kc, s:s + 1])

        import os
        if not os.environ.get('K_SKIP_NORM'):
            stream(kb, xts, loads, comp, kb.sp)

        wv = A.w_in[li].rearrange("(kc p) n -> p kc n", p=128)
        pieces = [(j * 512, min(512, N_IN - j * 512)) for j in range(8)]
        loads = [(lambda t, c0=c0, w=w: [(t[:, :, 0:w], wv[:, :, c0:c0 + w])]) for (c0, w) in pieces]
        oc = [0]

        npc = int(os.environ.get('K_PIECES', '8'))
        pieces = pieces[:npc]
        loads = loads[:npc]
        evac_mode = os.environ.get('K_EVAC', 'both')

        def comp2(j, slot):
            c0, w = pieces[j]
            tbs = [(tb * 512, 512) for tb in range(4)]
            if (not last) or j in (3, 4):
                tbs.append((2048, 256))
            for (t0, n) in tbs:
                rd = [hTt[t0 // 128 + i] for i in range(n // 128)]
                for m0 in ([0, 128, 256, 384] if w == 512 else [0, 128, w - 128]):
                    mw = 128
                    ps = kb.next_ps()
                    terms = [(slot[:, kc, m0:m0 + mw], hT[:, kc, t0:t0 + n]) for kc in range(16)]
                    mm(kb, ps[0:mw, 0:n], ps, terms, reads=[slot] + rd)
                    ot = ots[oc[0] % 4]
                    cp(kb, (kb.act if oc[0] % 2 else kb.dve) if evac_mode == 'both' else kb.dve, ot[0:mw, 0:n], ps[0:mw, 0:n], reads=[ps], writes=[ot])
                    oc[0] += 1
                    kb.dma(kb.sp, [(A.pT[c0 + m0:c0 + m0 + mw, t0:t0 + n], ot[0:mw, 0:n])], src=ot)

        if not os.environ.get('K_SKIP_PROJ'):
            stream(kb, ring, loads, comp2, kb.pool)
        kb.barrier()


IN_SPECS = [
    ("x", [L, D], F32), ("c", [16, 128], F32), ("ctx", [LC, D], F32), ("c_ctx", [16, 128], F32),
    ("norm1_g", [2, 16, 128], F32), ("norm2_g", [2, 16, 128], F32),
    ("w_ada", [2, D, 6 * D], F32), ("b_ada", [2, 96, 128], F32), ("b_ada_row", [2, 1, 6 * D], F32),
    ("w_in", [2, D, N_IN], F32),
    ("conv_dw_w", [2, 124, 128], F32), ("conv_dw_b", [2, 4, 128], F32), ("conv_ln_g", [2, 4, 128], F32),
    ("conv_ln_b", [2, 4, 128], F32), ("q_norm_g", [2, 6, 128], F32), ("w_uq", [2, 768, 1536], F32),
    ("kv_norm_g", [2, 4, 128], F32), ("w_ukv", [2, 512, 2048], F32),
    ("hy_short_w", [2, 36, 128], F32), ("hy_short_b", [2, 12, 128], F32),
    ("hy_w1", [2, 33, 64], F32), ("hy_b1", [2, 64, 1], F32), ("hy_freq1", [2, 64, 1], F32),
    ("hy_w2", [2, 64, 64], F32), ("hy_b2", [2, 64, 1], F32), ("hy_freq2", [2, 64, 1], F32),
    ("hy_w3", [2, 64, 2048], F32), ("hy_b3", [2, 1, 2048], F32), ("hy_bias", [2, 2, 512], F32),
    ("w_out", [2, D, D], F32), ("w_router", [D, NE], F32), ("router_bias", [1, NE], F32),
    ("w_gate", [2, NE, D, DFF], F32), ("w_up", [2, NE, D, DFF], F32), ("w_down", [2, NE, DFF, D], F32),
    ("final_norm_g", [1, D], F32),
]
CONST_DT = {"identb": BF16, "onesb": BF16, "perm": BF16, "dftc_l": BF16, "dfts_l": BF16, "dftc_c": BF16,
            "dfts_c": BF16, "alt_l": BF16, "alt_c": BF16, "altr_l": BF16, "altr_c": BF16}
SCRATCH = [
    ("gates", [2, 2, 2, D], F32),
    ("pT", [N_IN, T], BF16),
    ("olT", [D, T], BF16),
    ("kTn", [NH, 128, T], BF16), ("krT", [128, T], BF16), ("vtm", [T, NH * 128], BF16),
    ("qTn", [NH, 128, T], BF16), ("qTr", [NH, 128, T], BF16), ("uTd", [1536, T], BF16),
    ("xs", [L, D], F32), ("xcs", [LC, D], F32), ("h2Td", [D, T], BF16), ("combd", [T, NE], F32),
]


def build(consts, dbg=(), stop_after=None, skip=()):
    nc = bass.Bass("TRN2", target_bir_lowering=False)
    A = NS()
    for name, shape, dt in IN_SPECS:
        if name in skip:
            continue
        setattr(A, name, nc.dram_tensor(name, shape, dt, kind="ExternalInput").ap())
    Cd = NS()
    for name, arr in consts.items():
        setattr(Cd, name, nc.dram_tensor("k_" + name, list(arr.shape), CONST_DT.get(name, F32), kind="ExternalInput").ap())
    for name, shape, dt in SCRATCH:
        kind = "ExternalOutput" if name in dbg else "Internal"
        setattr(A, name, nc.dram_tensor(name, shape, dt, kind=kind).ap())
    A.out = nc.dram_tensor("out", [L, D], F32, kind="ExternalOutput").ap()

    with ExitStack() as es:
        kb = KB(nc, es)
        kb.init_psum(es)
        C = NS()
        C.ident32 = kb.sb(es, "ident32", [128, 128], F32)
        C.identb = kb.sb(es, "identb", [128, 128], BF16)
        C.ones32 = kb.sb(es, "ones32", [128, 128], F32)
        C.onesb = kb.sb(es, "onesb", [128, 128], BF16)
        C.perm = kb.sb(es, "perm", [64, 64], BF16)
        for nm in ("ident32", "identb", "ones32", "onesb", "perm"):
            t = getattr(C, nm)
            kb.dma(kb.sp, [(t[:, :], getattr(Cd, nm))], dst=t)
        C.d = Cd
        kb.barrier()
        xsrc, xcsrc = A.x, A.ctx
        done = False
        for li in range(DEPTH):
            with ExitStack() as lst:
                P = phase_params(kb, A, C, li, lst)
                if stop_after == ("params", li):
                    done = True
                    break
                phase_inproj(kb, A, C, P, li, xsrc, xcsrc)
                if stop_after == ("inproj", li):
                    done = True
                    break
                import os
                skipm = os.environ.get("K_SKIPM", "")
                if "c" not in skipm:
                    phase_conv(kb, A, C, P, li)
                if "a" not in skipm:
                    phase_qkv(kb, A, C, P, li)
                    phase_attn(kb, A, C, P, li)
                if "h" not in skipm:
                    phase_hyena(kb, A, C, P, li, 0, L, "l")
                    if li < DEPTH - 1:
                        phase_hyena(kb, A, C, P, li, L, LC, "c")
                if stop_after == ("mix", li):
                    done = True
                    break
                phase_outproj(kb, A, C, P, li, xsrc, xcsrc)
                xsrc, xcsrc = A.xs, A.xcs
                if stop_after == ("outproj", li):
                    done = True
                    break
                phase_moe(kb, A, C, P, li)
                if stop_after == ("moe", li):
                    done = True
                    break
        kb.barrier()
        print("instructions:", kb.n_inst)
    return nc


def prep_inputs(inputs, consts, b):
    f = lambda a: np.ascontiguousarray(np.asarray(a, dtype=np.float32))
    g = inputs
    m = {
        "x": f(g["x"][b]), "c": f(g["c"][b]).reshape(16, 128), "ctx": f(g["ctx"][b]),
        "c_ctx": f(g["c_ctx"]).reshape(16, 128),
        "norm1_g": f(g["norm1_g"]).reshape(2, 16, 128), "norm2_g": f(g["norm2_g"]).reshape(2, 16, 128),
        "w_ada": f(g["w_ada"]), "b_ada": f(g["b_ada"]).reshape(2, 96, 128), "b_ada_row": f(g["b_ada"]).reshape(2, 1, 6 * D),
        "w_in": f(g["w_in"]),
        "conv_dw_w": f(g["conv_dw_w"]).reshape(2, 124, 128), "conv_dw_b": f(g["conv_dw_b"]).reshape(2, 4, 128),
        "conv_ln_g": f(g["conv_ln_g"]).reshape(2, 4, 128), "conv_ln_b": f(g["conv_ln_b"]).reshape(2, 4, 128),
        "q_norm_g": f(g["q_norm_g"]).reshape(2, 6, 128), "w_uq": f(g["w_uq"]),
        "kv_norm_g": f(g["kv_norm_g"]).reshape(2, 4, 128), "w_ukv": f(g["w_ukv"]),
        "hy_short_w": f(g["hy_short_w"]).reshape(2, 36, 128), "hy_short_b": f(g["hy_short_b"]).reshape(2, 12, 128),
        "hy_w1": f(g["hy_w1"]), "hy_b1": f(g["hy_b1"]).reshape(2, 64, 1), "hy_freq1": f(g["hy_freq1"]).reshape(2, 64, 1),
        "hy_w2": f(g["hy_w2"]), "hy_b2": f(g["hy_b2"]).reshape(2, 64, 1), "hy_freq2": f(g["hy_freq2"]).reshape(2, 64, 1),
        "hy_w3": f(g["hy_w3"]), "hy_b3": f(g["hy_b3"]).reshape(2, 1, 2048), "hy_bias": f(g["hy_bias"]),
        "w_out": f(g["w_out"]), "w_router": f(g["w_router"]), "router_bias": f(g["router_bias"]).reshape(1, NE),
        "w_gate": f(g["w_gate"]), "w_up": f(g["w_up"]), "w_down": f(g["w_down"]),
        "final_norm_g": f(g["final_norm_g"]).reshape(1, D),
    }
    for k, v in consts.items():
        m["k_" + k] = v
    return m


_CACHE = {}


def kernel(**inputs):
    consts = _CACHE.get("consts")
    if consts is None:
        consts = _CACHE["consts"] = make_consts()
    nc = build(consts)
    in_maps = [prep_inputs(inputs, consts, b) for b in range(8)]
    res = run_bass_kernel_spmd(nc, in_maps, core_ids=list(range(8)))
    return np.stack([np.asarray(r["out"], dtype=np.float32) for r in res.results], axis=0)


def phase_outproj(kb, A, C, P, li, xsrc, xcsrc):
    last = li == DEPTH - 1
    ntile = 16 if last else 18
    BIG = 1.0e4
    with ExitStack() as st:
        wout = kb.sb(st, "wout", [128, 16, D], BF16)
        wv = A.w_out[li].rearrange("(kc p) n -> p kc n", p=128)
        wsubs = []
        for q4 in range(4):
            sub_ = kb.sub(wout.ap[:, q4 * 4:(q4 + 1) * 4, :], f"wout{q4}")
            kb.dma(kb.pool, [(sub_[:, :, :], wv[:, q4 * 4:(q4 + 1) * 4, :])], dst=sub_)
            wsubs.append(sub_)
        gbc = [kb.sb(st, f"gbc{s}", [128, D], F32) for s in range(2)]
        for s in range(2):
            kb.dma(kb.sp, [(gbc[s][:, :], A.gates[li, 0, s:s + 1, :].partition_broadcast(128))], dst=gbc[s])
        wr = kb.sb(st, "wr", [128, 16, NE], F32)
        kb.dma(kb.sp, [(wr[:, :, :], A.w_router.rearrange("(kc p) e -> p kc e", p=128))], dst=wr)
        rb = kb.sb(st, "rb", [128, NE], F32)
        kb.dma(kb.sp, [(rb[:, :], A.router_bias.partition_broadcast(128))], dst=rb)
        ols = [kb.sb(st, f"ol{i}", [128, 16, 512], BF16) for i in range(2)]
        xts = [kb.sb(st, f"xt{i}", [128, D], F32) for i in range(2)]
        xos = [kb.sb(st, f"xo{i}", [128, D], F32) for i in range(2)]
        xns = [kb.sb(st, f"xn{i}", [128, D], F32) for i in range(2)]
        h32s = [kb.sb(st, f"h32{i}", [128, 16, 128], F32) for i in range(2)]
        hbs = [kb.sb(st, f"hb{i}", [128, 16, 128], BF16) for i in range(2)]
        cbs = [kb.sb(st, f"cb{i}", [128, NE], F32) for i in range(2)]
        junk = kb.sb(st, "junk", [128, D], BF16)
        sss = [kb.sb(st, f"ss{i}", [128, 1], F32) for i in range(2)]
        rss = [kb.sb(st, f"rs{i}", [128, 1], F32) for i in range(2)]
        r_sc = kb.sb(st, "r_sc", [128, NE], F32)
        r_sel = kb.sb(st, "r_sel", [128, NE], F32)
        r_t = kb.sb(st, "r_t", [128, NE], F32)
        r_m = kb.sb(st, "r_m", [128, 8], F32)
        r_g = kb.sb(st, "r_g", [128, 8], F32)
        olv = A.olT.rearrange("(kc p) t -> p kc t", p=128)
        h2v = A.h2Td.rearrange("(kc p) t -> p kc t", p=128)

        def xrows(ti):
            return xsrc[ti * 128:(ti + 1) * 128, :] if ti < 16 else xcsrc[(ti - 16) * 128:(ti - 15) * 128, :]

        def load_ol(b):
            t0_ = b * 4
            nbt = min(4, ntile - t0_)
            o = ols[b % 2]
            kb.dma(kb.sp, [(o[:, :, 0:nbt * 128], olv[:, :, t0_ * 128:(t0_ + nbt) * 128])], dst=o)

        def load_x(ti):
            kb.dma(kb.sp, [(xts[ti % 2][:, :], xrows(ti))], dst=xts[ti % 2])

        load_ol(0)
        load_x(0)

        def stage1(ti):
            s = 0 if ti < 16 else 1
            b = ti // 4
            if ti % 4 == 0 and (b + 1) * 4 < ntile:
                load_ol(b + 1)
            if ti + 1 < ntile:
                load_x(ti + 1)
            ol, xt, xo, xn = ols[b % 2], xts[ti % 2], xos[ti % 2], xns[ti % 2]
            h32, hb, cb, ss, rs = h32s[ti % 2], hbs[ti % 2], cbs[ti % 2], sss[ti % 2], rss[ti % 2]
            c0 = (ti % 4) * 128
            orow = A.xs[ti * 128:(ti + 1) * 128, :] if ti < 16 else A.xcs[(ti - 16) * 128:(ti - 15) * 128, :]
            for db in range(4):
                ps = kb.next_ps()
                terms = [(ol[:, kc, c0:c0 + 128], wout[:, kc, db * 512:(db + 1) * 512]) for kc in range(16)]
                mm(kb, ps[:, :], ps, terms, reads=[ol] + wsubs)
                tt(kb, kb.dve, xo[:, db * 512:(db + 1) * 512], ps[:, :], gbc[s][:, db * 512:(db + 1) * 512], ALU.mult,
                   reads=[ps, gbc[s]], writes=[xo])
            tt(kb, kb.dve, xo[:, :], xo[:, :], xt[:, :], ALU.add, reads=[xo, xt], writes=[xo])
            kb.dma(kb.pool, [(orow, xo[:, :])], src=xo)
            tok_rstd(kb, xo, junk, ss, rs, D)
            ts(kb, kb.dve, xn[:, :], xo[:, :], rs[:, 0:1], None, ALU.mult, None, reads=[xo, rs], writes=[xn])

        def stage2(ti):
            s = 0 if ti < 16 else 1
            xn = xns[ti % 2]
            h32, hb, cb = h32s[ti % 2], hbs[ti % 2], cbs[ti % 2]
            for g in range(4):
                ps = kb.next_ps()
                for k4 in range(4):
                    kc = g * 4 + k4
                    tr(kb, ps[:, k4 * 128:(k4 + 1) * 128], ps, xn[:, kc * 128:(kc + 1) * 128], C.ident32[:, :],
                       reads=[xn, C.ident32])
                for k4 in range(4):
                    kc = g * 4 + k4
                    if g % 2 == 0:
                        ts(kb, kb.dve, h32[:, kc, :], ps[:, k4 * 128:(k4 + 1) * 128], P.g2m[:, kc, s:s + 1],
                           P.mod[:, 48 + kc, s:s + 1], ALU.mult, ALU.add, reads=[ps, P.g2m, P.mod], writes=[h32])
                    else:
                        act(kb, h32[:, kc, :], ps[:, k4 * 128:(k4 + 1) * 128], AF.Identity, reads=[ps, P.g2m, P.mod], writes=[h32],
                            bias=P.mod[:, 48 + kc, s:s + 1], scale=P.g2m[:, kc, s:s + 1])
            cp(kb, kb.act, hb[:, :, :], h32[:, :, :], reads=[h32], writes=[hb])
            kb.dma(kb.sp, [(h2v[:, :, ti * 128:(ti + 1) * 128], hb[:, :, :])], src=hb)
        def stage3(ti):
            s = 0 if ti < 16 else 1
            h32, cb = h32s[ti % 2], cbs[ti % 2]
            ps = kb.next_ps()
            mm(kb, ps[:, 0:NE], ps, [(h32[:, kc, :], wr[:, kc, :]) for kc in range(16)], reads=[h32, wr])
            act(kb, r_sc[:, :], ps[:, 0:NE], AF.Sigmoid, reads=[ps], writes=[r_sc])
            tt(kb, kb.dve, r_sel[:, :], r_sc[:, :], rb[:, :], ALU.add, reads=[r_sc, rb], writes=[r_sel])
            sel3 = r_sel[:, :].rearrange("p (g j) -> p g j", g=4)
            t3 = r_t[:, :].rearrange("p (g j) -> p g j", g=4)
            kb.op(kb.dve, lambda e: e.tensor_reduce(out=r_m[:, 0:4], in_=sel3, axis=AX.X, op=ALU.max),
                  reads=[r_sel], writes=[r_m])
            for g in range(4):
                ts(kb, kb.dve, t3[:, g, :], sel3[:, g, :], r_m[:, g:g + 1], -BIG, ALU.is_ge, ALU.mult,
                   reads=[r_sel, r_m], writes=[r_t])
            tt(kb, kb.dve, r_t[:, :], r_t[:, :], r_sel[:, :], ALU.add, reads=[r_t, r_sel], writes=[r_t])
            kb.op(kb.dve, lambda e: e.tensor_reduce(out=r_m[:, 4:8], in_=t3, axis=AX.X, op=ALU.max),
                  reads=[r_t], writes=[r_m])
            tt(kb, kb.dve, r_g[:, 0:4], r_m[:, 0:4], r_m[:, 4:8], ALU.add, reads=[r_m], writes=[r_g])
            kb.op(kb.dve, lambda e: e.tensor_reduce(out=r_g[:, 4:5], in_=r_g[:, 0:4], axis=AX.X, op=ALU.max),
                  reads=[r_g], writes=[r_g])
            ts(kb, kb.dve, r_g[:, 0:4], r_g[:, 0:4], r_g[:, 4:5], None, ALU.is_ge, None, reads=[r_g], writes=[r_g])
            for g in range(4):
                ts(kb, kb.dve, t3[:, g, :], sel3[:, g, :], r_g[:, g:g + 1], None, ALU.mult, None,
                   reads=[r_sel, r_g], writes=[r_t])
                ts(kb, kb.dve, t3[:, g, :], t3[:, g, :], r_g[:, g:g + 1], None, ALU.add, None,
                   reads=[r_t, r_g], writes=[r_t])
            kb.op(kb.dve, lambda e: e.tensor_reduce(out=r_m[:, 0:1], in_=r_t[:, :], axis=AX.X, op=ALU.max),
                  reads=[r_t], writes=[r_m])
            ts(kb, kb.dve, r_sel[:, :], r_t[:, :], r_m[:, 0:1], -BIG, ALU.is_ge, ALU.mult, reads=[r_t, r_m], writes=[r_sel])
            tt(kb, kb.dve, r_sel[:, :], r_sel[:, :], r_t[:, :], ALU.add, reads=[r_sel, r_t], writes=[r_sel])
            kb.op(kb.dve, lambda e: e.tensor_reduce(out=r_m[:, 1:2], in_=r_sel[:, :], axis=AX.X, op=ALU.max),
                  reads=[r_sel], writes=[r_m])
            ts(kb, kb.dve, r_t[:, :], r_t[:, :], r_m[:, 1:2], None, ALU.is_ge, None, reads=[r_t, r_m], writes=[r_t])
            tt(kb, kb.dve, r_t[:, :], r_t[:, :], r_sc[:, :], ALU.mult, reads=[r_t, r_sc], writes=[r_t])
            kb.op(kb.dve, lambda e: e.tensor_reduce(out=r_m[:, 2:3], in_=r_t[:, :], axis=AX.X, op=ALU.add),
                  reads=[r_t], writes=[r_m])
            kb.op(kb.dve, lambda e: e.reciprocal(out=r_m[:, 2:3], in_=r_m[:, 2:3]), reads=[r_m], writes=[r_m])
            ts(kb, kb.dve, cb[:, :], r_t[:, :], r_m[:, 2:3], None, ALU.mult, None, reads=[r_t, r_m], writes=[cb])
            kb.dma(kb.sp, [(A.combd[ti * 128:(ti + 1) * 128, :], cb[:, :])], src=cb)

        for ti in range(ntile + 2):
            if ti < ntile:
                stage1(ti)
            if 1 <= ti <= ntile:
                stage2(ti - 1)
            if ti >= 2:
                stage3(ti - 2)
        kb.barrier()


def phase_moe(kb, A, C, P, li):
    last = li == DEPTH - 1
    ntile = 16 if last else 18
    blocks = []
    t0 = 0
    sizes = [9, 9] if ntile == 18 else [8, 8]
    NBM = max(sizes)
    for nb in sizes:
        blocks.append((t0, nb))
        t0 += nb
    assert t0 == ntile
    BIG = 1.0e4
    with ExitStack() as st:
        gbc = [kb.sb(st, f"gbc{s}", [128, D], F32) for s in range(2)]
        for s in range(2):
            kb.dma(kb.sp, [(gbc[s][:, :], A.gates[li, 1, s:s + 1, :].partition_broadcast(128))], dst=gbc[s])
        fg = None
        if last:
            fg = kb.sb(st, "fg", [128, D], F32)
            kb.dma(kb.sp, [(fg[:, :], A.final_norm_g.partition_broadcast(128))], dst=fg)
        h2T = kb.sb(st, "h2T", [128, 16, NBM * 128], BF16)
        heT = [kb.sb(st, "heT0", [128, 8, NBM * 128], BF16)] * 2
        yacc = kb.sb(st, "yacc", [128, NBM, D], F32)
        yat = [kb.sub(yacc.ap[:, i, :], f"ya{i}") for i in range(NBM)]
        comb = kb.sb(st, "comb", [128, NBM, NE], F32)
        ring = [kb.sb(st, f"wr{i}", [128, 16, 256], BF16) for i in range(5 if last else 4)]
        xt = kb.sb(st, "xt", [128, D], F32)
        xn = kb.sb(st, "xn", [128, D], F32)
        junk = kb.sb(st, "junk", [128, D], BF16)
        ss = kb.sb(st, "ss", [128, 1], F32)
        rs = kb.sb(st, "rs", [128, 1], F32)
        sg = [kb.sb(st, f"sg{i}", [128, 512], F32) for i in range(2)]

        def xrows(ti, src=True):
            if ti < 16:
                base = (A.xs if True else None)
                return A.xs[ti * 128:(ti + 1) * 128, :]
            return A.xcs[(ti - 16) * 128:(ti - 15) * 128, :]

        for (tb0, nb) in blocks:
            ntok = nb * 128
            h2v = A.h2Td.rearrange("(kc p) t -> p kc t", p=128)
            kb.dma(kb.sp, [(h2T[:, :, 0:ntok], h2v[:, :, tb0 * 128:tb0 * 128 + ntok])], dst=h2T)
            kb.dma(kb.sp, [(comb[:, i, :], A.combd[(tb0 + i) * 128:(tb0 + i + 1) * 128, :]) for i in range(nb)], dst=comb)
            nch = (ntok + 511) // 512
            csz = ntok // nch
            nsub = [(o, csz) for o in range(0, ntok, csz)]
            loads = []
            meta = []
            for e in range(NE):
                for f4 in range(4):
                    wg = A.w_gate[li, e].rearrange("(kc p) n -> p kc n", p=128)[:, :, f4 * 256:(f4 + 1) * 256]
                    wu = A.w_up[li, e].rearrange("(kc p) n -> p kc n", p=128)[:, :, f4 * 256:(f4 + 1) * 256]
                    loads.append(lambda t, wg=wg: [(t[:, :, :], wg)])
                    meta.append(("g", e, f4))
                    loads.append(lambda t, wu=wu: [(t[:, :, :], wu)])
                    meta.append(("u", e, f4))
                for d4 in range(4):
                    wd = A.w_down[li, e].rearrange("(fc p) n -> p fc n", p=128)[:, :, d4 * 512:(d4 + 1) * 512]
                    loads.append(lambda t, wd=wd: [(t[:, :, :].rearrange("p a b -> p (a b)").rearrange("p (f n) -> p f n", f=8), wd)])
                    meta.append(("d", e, d4))
            state = {}
            sgc = [0]

            def comp(j, slot):
                kind, e, idx = meta[j]
                he = heT[e % 2]
                if kind == "g":
                    state["g"] = slot
                    return
                if kind == "u":
                    gs = state["g"]
                    for (o, n) in nsub:
                        for m in range(2):
                            ffc = idx * 2 + m
                            psg = kb.next_ps()
                            mm(kb, psg[:, 0:n], psg, [(gs[:, kc, m * 128:(m + 1) * 128], h2T[:, kc, o:o + n]) for kc in range(16)],
                               reads=[gs, h2T])
                            psu = kb.next_ps()
                            mm(kb, psu[:, 0:n], psu, [(slot[:, kc, m * 128:(m + 1) * 128], h2T[:, kc, o:o + n]) for kc in range(16)],
                               reads=[slot, h2T])
                            sgt = sg[sgc[0] % 2]
                            sgc[0] += 1
                            act(kb, sgt[:, 0:n], psg[:, 0:n], AF.Silu, reads=[psg], writes=[sgt])
                            tt(kb, kb.dve, he[:, ffc, o:o + n], psu[:, 0:n], sgt[:, 0:n], ALU.mult, reads=[psu, sgt], writes=[he])
                    return
                wdv = slot[:, :, :].rearrange("p a b -> p (a b)").rearrange("p (f n) -> p f n", f=8)
                for i in range(nb):
                    ps = kb.next_ps()
                    mm(kb, ps[:, :], ps, [(he[:, fc, i * 128:(i + 1) * 128], wdv[:, fc, :]) for fc in range(8)], reads=[he, slot])
                    o_ = yat[i][:, idx * 512:(idx + 1) * 512]
                    if e == 0:
                        ts(kb, kb.dve, o_, ps[:, :], comb[:, i, e:e + 1], None, ALU.mult, None, reads=[ps, comb], writes=[yat[i]])
                    else:
                        stt(kb, kb.dve, o_, ps[:, :], comb[:, i, e:e + 1], o_, ALU.mult, ALU.add, reads=[ps, comb, yat[i]], writes=[yat[i]])

            stream(kb, ring, loads, comp, kb.pool, keep=1)
            for i in range(nb):
                ti = tb0 + i
                s = 0 if ti < 16 else 1
                kb.dma(kb.sp, [(xt[:, :], xrows(ti))], dst=xt)
                tt(kb, kb.dve, xn[:, :], yat[i][:, :], gbc[s][:, :], ALU.mult, reads=[yat[i], gbc[s]], writes=[xn])
                tt(kb, kb.dve, xn[:, :], xn[:, :], xt[:, :], ALU.add, reads=[xn, xt], writes=[xn])
                if last:
                    tok_rstd(kb, xn, junk, ss, rs, D)
                    stt(kb, kb.dve, xn[:, :], xn[:, :], rs[:, 0:1], fg[:, :], ALU.mult, ALU.mult, reads=[xn, rs, fg], writes=[xn])
                    kb.dma(kb.sp, [(A.out[ti * 128:(ti + 1) * 128, :], xn[:, :])], src=xn)
                else:
                    kb.dma(kb.sp, [(xrows(ti), xn[:, :])], src=xn)
        kb.barrier()


def fm_rstd(kb, C, src, nk, ncols, width, rstd, sq):
    for c0 in range(0, ncols, 512):
        n = min(512, ncols - c0)
        for kc in range(nk):
            act(kb, sq[:, kc, 0:n], src[:, kc, c0:c0 + n], AF.Square, reads=[src], writes=[sq])
        ps = kb.next_ps()
        mm(kb, ps[:, 0:n], ps, [(C.onesb[:, :], sq[:, kc, 0:n]) for kc in range(nk)], reads=[C.onesb, sq])
        act(kb, rstd[:, c0:c0 + n], ps[:, 0:n], AF.Sqrt, reads=[ps], writes=[rstd], bias=EPS, scale=1.0 / width)
        kb.op(kb.dve, lambda e, c0=c0, n=n: e.reciprocal(out=rstd[:, c0:c0 + n], in_=rstd[:, c0:c0 + n]), reads=[rstd], writes=[rstd])


def rope_apply(kb, C, a_sb, n, c0, rcos, rsin, out_ap, out_tile, tmp1, tmp2):
    ps = kb.next_ps()
    mm(kb, ps[0:64, 0:n], ps, [(C.perm[:, :], a_sb[0:64, 0:n])], reads=[C.perm, a_sb])
    tt(kb, kb.dve, tmp1[0:64, 0:n], a_sb[0:64, 0:n], rcos[0:64, c0:c0 + n], ALU.mult, reads=[a_sb, rcos], writes=[tmp1])
    tt(kb, kb.dve, tmp2[0:64, 0:n], ps[0:64, 0:n], rsin[0:64, c0:c0 + n], ALU.mult, reads=[ps, rsin], writes=[tmp2])
    tt(kb, kb.dve, out_ap, tmp1[0:64, 0:n], tmp2[0:64, 0:n], ALU.add, reads=[tmp1, tmp2], writes=[out_tile])


TBLK = [(0, 512), (512, 512), (1024, 512), (1536, 512), (2048, 256)]


def phase_qkv(kb, A, C, P, li):
    last = li == DEPTH - 1
    with ExitStack() as st:
        rcos = kb.sb(st, "rcos", [64, L], F32)
        rsin = kb.sb(st, "rsin", [64, L], F32)
        kb.dma(kb.sp, [(rcos[:, :], C.d.rcos)], dst=rcos)
        kb.dma(kb.sp, [(rsin[:, :], C.d.rsin)], dst=rsin)
        rstd = kb.sb(st, "rstd", [128, T], F32)
        sq = kb.sb(st, "sq", [128, 6, 512], BF16)
        tmp1s = [kb.sb(st, f"tmp1{i}", [64, 512], F32) for i in range(2)]
        tmp2s = [kb.sb(st, f"tmp2{i}", [64, 512], F32) for i in range(2)]
        asbs = [kb.sb(st, f"asb{i}", [64, 512], BF16) for i in range(2)]
        rcnt = [0]

        def rope_bufs():
            i = rcnt[0] % 2
            rcnt[0] += 1
            return asbs[i], tmp1s[i], tmp2s[i]
        ots = [kb.sb(st, f"ot{i}", [128, 512], BF16) for i in range(4)]
        oc = [0]

        def evac_store(ps, n, dram_ap):
            ot = ots[oc[0] % 4]
            cp(kb, kb.act if oc[0] % 2 else kb.dve, ot[:, 0:n], ps[:, 0:n], reads=[ps], writes=[ot])
            oc[0] += 1
            kb.dma(kb.sp, [(dram_ap, ot[:, 0:n])], src=ot)

        with ExitStack() as s2:
            pk = kb.sb(s2, "pk", [128, 4, T], BF16)
            kr = kb.sb(s2, "kr", [64, T], BF16)
            ckv = kb.sb(s2, "ckv", [128, 4, T], BF16)
            wk = kb.sb(s2, "wk", [128, 4, NH, 128], BF16)
            wvv = kb.sb(s2, "wvv", [128, 4, NH, 128], BF16)
            kro = kb.sb(s2, "kro", [128, T], BF16)
            kb.dma(kb.sp, [(pk[:, :, :], A.pT[OFF_KV:OFF_KV + 512, :].rearrange("(kc p) t -> p kc t", p=128))], dst=pk)
            kb.dma(kb.sp, [(kr[:, :], A.pT[OFF_KV + 512:OFF_KV + 576, :])], dst=kr)
            wsrc = A.w_ukv[li].rearrange("(kc p) (h t d) -> p kc h t d", p=128, h=NH, t=2)
            kb.dma(kb.pool, [(wk[:, kc, :, :], wsrc[:, kc, :, 0, :]) for kc in range(4)], dst=wk)
            kb.dma(kb.pool, [(wvv[:, kc, :, :], wsrc[:, kc, :, 1, :]) for kc in range(4)], dst=wvv)
            fm_rstd(kb, C, pk, 4, T, 512, rstd, sq)
            for kc in range(4):
                stt(kb, kb.dve, ckv[:, kc, :], pk[:, kc, :], P.pa[:, 82 + kc:83 + kc], rstd[:, :], ALU.mult, ALU.mult,
                    reads=[pk, P.pa, rstd], writes=[ckv])
            for h in range(NH):
                for (t0, n) in TBLK:
                    ps = kb.next_ps()
                    mm(kb, ps[:, 0:n], ps, [(wk[:, kc, h, :], ckv[:, kc, t0:t0 + n]) for kc in range(4)], reads=[wk, ckv])
                    evac_store(ps, n, A.kTn[h, :, t0:t0 + n])
            wv2 = wvv[:, :, :, :].rearrange("p k h d -> p k (h d)")
            for ti in range(18):
                for hb in range(2):
                    ps = kb.next_ps()
                    mm(kb, ps[:, :], ps, [(ckv[:, kc, ti * 128:(ti + 1) * 128], wv2[:, kc, hb * 512:(hb + 1) * 512]) for kc in range(4)],
                       reads=[wvv, ckv])
                    evac_store(ps, 512, A.vtm[ti * 128:(ti + 1) * 128, hb * 512:(hb + 1) * 512])
            memset(kb, kb.dve, kro[:, :], 0.0, writes=[kro])
            for (t0, n) in TBLK[:4]:
                asb, tmp1, tmp2 = rope_bufs()
                cp(kb, kb.act, asb[:, 0:n], kr[:, t0:t0 + n], reads=[kr], writes=[asb])
                rope_apply(kb, C, asb, n, t0, rcos, rsin, kro[0:64, t0:t0 + n], kro, tmp1, tmp2)
            cp(kb, kb.dve, kro[0:64, L:T], kr[:, L:T], reads=[kr], writes=[kro])
            kb.dma(kb.sp, [(A.krT[:, :], kro[:, :])], src=kro)
            kb.barrier()
        with ExitStack() as s2:
            pq = kb.sb(s2, "pq", [128, 6, T], BF16)
            qn = kb.sb(s2, "qn", [128, 6, T], BF16)
            wq = kb.sb(s2, "wq", [128, 6, 1536], BF16)
            qro = [kb.sb(s2, f"qro{i}", [128, 512], BF16) for i in range(2)]
            kb.dma(kb.sp, [(pq[:, :, :], A.pT[OFF_Q:OFF_Q + 768, :].rearrange("(kc p) t -> p kc t", p=128))], dst=pq)
            kb.dma(kb.pool, [(wq[:, :, :], A.w_uq[li].rearrange("(kc p) n -> p kc n", p=128))], dst=wq)
            fm_rstd(kb, C, pq, 6, T, 768, rstd, sq)
            for kc in range(6):
                stt(kb, kb.dve, qn[:, kc, :], pq[:, kc, :], P.pa[:, 76 + kc:77 + kc], rstd[:, :], ALU.mult, ALU.mult,
                    reads=[pq, P.pa, rstd], writes=[qn])
            for i in range(2):
                memset(kb, kb.dve, qro[i][:, :], 0.0, writes=[qro[i]])
            blks = TBLK[:4] if last else TBLK
            rc = [0]
            for h in range(NH):
                for (t0, n) in blks:
                    ps = kb.next_ps()
                    mm(kb, ps[:, 0:n], ps, [(wq[:, kc, h * 192:h * 192 + 128], qn[:, kc, t0:t0 + n]) for kc in range(6)], reads=[wq, qn])
                    evac_store(ps, n, A.qTn[h, :, t0:t0 + n])
                    ps = kb.next_ps()
                    mm(kb, ps[0:64, 0:n], ps, [(wq[:, kc, h * 192 + 128:h * 192 + 192], qn[:, kc, t0:t0 + n]) for kc in range(6)], reads=[wq, qn])
                    qo = qro[rc[0] % 2]
                    rc[0] += 1
                    if t0 < L:
                        asb, tmp1, tmp2 = rope_bufs()
                        cp(kb, kb.act, asb[:, 0:n], ps[0:64, 0:n], reads=[ps], writes=[asb])
                        rope_apply(kb, C, asb, n, t0, rcos, rsin, qo[0:64, 0:n], qo, tmp1, tmp2)
                    else:
                        cp(kb, kb.act, qo[0:64, 0:n], ps[0:64, 0:n], reads=[ps], writes=[qo])
                    kb.dma(kb.sp, [(A.qTr[h, :, t0:t0 + n], qo[:, 0:n])], src=qo)
            kb.barrier()
        kb.barrier()


def phase_attn(kb, A, C, P, li):
    last = li == DEPTH - 1
    with ExitStack() as st:
        krt = kb.sb(st, "krt", [128, T], BF16)
        kb.dma(kb.sp, [(krt[:, :], A.krT)], dst=krt)
        kts = [kb.sb(st, f"kt{i}", [128, T], BF16) for i in range(2)]
        qns = [kb.sb(st, f"qn{i}", [128, T], BF16) for i in range(2)]
        qrs = [kb.sb(st, f"qr{i}", [128, T], BF16) for i in range(2)]
        vhs = [kb.sb(st, f"vh{i}", [128, 18, 128], BF16) for i in range(2)]
        pts = [kb.sb(st, f"pt{i}", [128, 18, 512], BF16) for i in range(2)]
        recs = [kb.sb(st, f"rec{i}", [128, 512], F32) for i in range(2)]
        ots = [kb.sb(st, f"ot{i}", [128, 512], BF16) for i in range(2)]
        pc = [0]
        vsrc = A.vtm.rearrange("(t p) d -> p t d", p=128)

        def load_head(h):
            i = h % 2
            kb.dma(kb.sp, [(kts[i][:, :], A.kTn[h])], dst=kts[i])
            kb.dma(kb.sp, [(qns[i][:, :], A.qTn[h])], dst=qns[i])
            kb.dma(kb.sp, [(qrs[i][:, :], A.qTr[h])], dst=qrs[i])
            kb.dma(kb.sp, [(vhs[i][:, :, :], vsrc[:, :, h * 128:(h + 1) * 128])], dst=vhs[i])

        load_head(0)
        for h in range(NH):
            if h + 1 < NH:
                load_head(h + 1)
            i = h % 2
            kt, qn, qr, vh = kts[i], qns[i], qrs[i], vhs[i]
            jobs = [(t0, n, list(range(18))) for (t0, n) in TBLK[:4]]
            if not last:
                jobs.append((L, 256, [16, 17]))
            for (t0, n, chunks) in jobs:
                pt = pts[pc[0] % 2]
                ot = ots[pc[0] % 2]
                rec = recs[pc[0] % 2]
                pc[0] += 1
                for ci, ch in enumerate(chunks):
                    ps = kb.next_ps()
                    mm(kb, ps[:, 0:n], ps, [(kt[:, ch * 128:(ch + 1) * 128], qn[:, t0:t0 + n]),
                                            (krt[:, ch * 128:(ch + 1) * 128], qr[:, t0:t0 + n])], reads=[kt, qn, krt, qr])
                    act(kb, pt[:, ci, 0:n], ps[:, 0:n], AF.Exp, reads=[ps], writes=[pt], scale=SCALE)
                pso = kb.next_ps()
                mm(kb, pso[:, 0:n], pso, [(vh[:, ch, :], pt[:, ci, 0:n]) for ci, ch in enumerate(chunks)], reads=[vh, pt])
                pss = kb.next_ps()
                mm(kb, pss[:, 0:n], pss, [(C.onesb[:, :], pt[:, ci, 0:n]) for ci, ch in enumerate(chunks)], reads=[C.onesb, pt])
                kb.op(kb.dve, lambda e, n=n, pss=pss, rec=rec: e.reciprocal(out=rec[:, 0:n], in_=pss[:, 0:n]), reads=[pss], writes=[rec])
                tt(kb, kb.dve, ot[:, 0:n], pso[:, 0:n], rec[:, 0:n], ALU.mult, reads=[pso, rec], writes=[ot])
                kb.dma(kb.sp, [(A.olT[512 + h * 128:512 + (h + 1) * 128, t0:t0 + n], ot[:, 0:n])], src=ot)
        kb.barrier()


def phase_conv(kb, A, C, P, li):
    last = li == DEPTH - 1
    seqs = [(0, L)] if last else [(0, L), (L, LC)]
    with ExitStack() as st:
        yc = kb.sb(st, "yc", [128, 4, T], F32)
        zl = kb.sb(st, "zl", [128, T + 60], BF16)
        dg = kb.sb(st, "dg", [128, 31, 128], BF16)
        ab = [kb.sb(st, f"ab{i}", [128, T], BF16) for i in range(2)]
        sgm = kb.sb(st, "sgm", [128, T], BF16)
        memset(kb, kb.dve, zl[:, :], 0.0, writes=[zl])
        zoff = {0: 15, L: L + 30 + 15}
        for cc in range(4):
            kb.dma(kb.sp, [(ab[0][:, :], A.pT[cc * 128:(cc + 1) * 128, :])], dst=ab[0])
            kb.dma(kb.sp, [(ab[1][:, :], A.pT[512 + cc * 128:512 + (cc + 1) * 128, :])], dst=ab[1])
            act(kb, sgm[:, :], ab[1][:, :], AF.Sigmoid, reads=[ab[1]], writes=[sgm])
            for (o, n) in seqs:
                tt(kb, kb.dve, zl[:, zoff[o]:zoff[o] + n], ab[0][:, o:o + n], sgm[:, o:o + n], ALU.mult, reads=[ab[0], sgm], writes=[zl])
            for k in range(31):
                ts(kb, kb.dve, dg[:, k, :], C.identb[:, :], P.pb[:, k * 4 + cc:k * 4 + cc + 1], None, ALU.mult, None,
                   reads=[C.identb, P.pb], writes=[dg])
            for (o, n) in seqs:
                for b0 in range(0, n, 512):
                    nn = min(512, n - b0)
                    ps = kb.next_ps()
                    z0 = zoff[o] - 15 + b0
                    mm(kb, ps[:, 0:nn], ps, [(dg[:, k, :], zl[:, z0 + k:z0 + k + nn]) for k in range(31)], reads=[dg, zl])
                    ts(kb, kb.dve, yc[:, cc, o + b0:o + b0 + nn], ps[:, 0:nn], P.pa[:, 64 + cc:65 + cc], None, ALU.add, None,
                       reads=[ps, P.pa], writes=[yc])
        ysq = kb.sb(st, "ysq", [128, 4, 512], F32)
        mean = kb.sb(st, "mean", [128, 512], F32)
        msq = kb.sb(st, "msq", [128, 512], F32)
        rstd = kb.sb(st, "rstd", [128, 512], F32)
        t1 = kb.sb(st, "t1", [128, 512], F32)
        ots = [kb.sb(st, f"ot{i}", [128, 512], BF16) for i in range(2)]
        oc = 0
        for (t0, n) in (TBLK[:4] if last else TBLK):
            for cc in range(4):
                act(kb, ysq[:, cc, 0:n], yc[:, cc, t0:t0 + n], AF.Square, reads=[yc], writes=[ysq])
            psm = kb.next_ps()
            mm(kb, psm[:, 0:n], psm, [(C.ones32[:, :], yc[:, cc, t0:t0 + n]) for cc in range(4)], reads=[C.ones32, yc])
            psq = kb.next_ps()
            mm(kb, psq[:, 0:n], psq, [(C.ones32[:, :], ysq[:, cc, 0:n]) for cc in range(4)], reads=[C.ones32, ysq])
            act(kb, mean[:, 0:n], psm[:, 0:n], AF.Copy, reads=[psm], writes=[mean], scale=1.0 / 512)
            tt(kb, kb.dve, msq[:, 0:n], mean[:, 0:n], mean[:, 0:n], ALU.mult, reads=[mean], writes=[msq])
            stt(kb, kb.dve, rstd[:, 0:n], psq[:, 0:n], 1.0 / 512, msq[:, 0:n], ALU.mult, ALU.subtract, reads=[psq, msq], writes=[rstd])
            act(kb, rstd[:, 0:n], rstd[:, 0:n], AF.Sqrt, reads=[rstd], writes=[rstd], bias=EPS, scale=1.0)
            kb.op(kb.dve, lambda e, n=n: e.reciprocal(out=rstd[:, 0:n], in_=rstd[:, 0:n]), reads=[rstd], writes=[rstd])
            for cc in range(4):
                tt(kb, kb.dve, t1[:, 0:n], yc[:, cc, t0:t0 + n], mean[:, 0:n], ALU.subtract, reads=[yc, mean], writes=[t1])
                tt(kb, kb.dve, t1[:, 0:n], t1[:, 0:n], rstd[:, 0:n], ALU.mult, reads=[t1, rstd], writes=[t1])
                ot = ots[oc % 2]
                oc += 1
                act(kb, ot[:, 0:n], t1[:, 0:n], AF.Silu, reads=[t1, P.pa], writes=[ot], bias=P.pa[:, 72 + cc:73 + cc],
                    scale=P.pa[:, 68 + cc:69 + cc])
                kb.dma(kb.sp, [(A.olT[cc * 128:(cc + 1) * 128, t0:t0 + n], ot[:, 0:n])], src=ot)
        kb.barrier()


def phase_hyena(kb, A, C, P, li, off, Ls, tag):
    nT = Ls // 128
    blks = [(b0, min(512, Ls - b0)) for b0 in range(0, Ls, 512)]
    iblk = [(b0, min(256, Ls - b0)) for b0 in range(0, Ls, 256)]
    dC = getattr(C.d, "dftc_" + tag)
    dS = getattr(C.d, "dfts_" + tag)
    inv_scale = 2.0 / (2 * Ls)
    with ExitStack() as st:
        pz = [kb.sb(st, f"pz{i}", [128, Ls + 2], BF16) for i in range(2)]
        uo = [kb.sb(st, f"uo{i}", [128, Ls], BF16) for i in range(2)]
        for i in range(2):
            memset(kb, kb.dve, pz[i][:, :], 0.0, writes=[pz[i]])
        for j in range(12):
            p_, u_ = pz[j % 2], uo[j % 2]
            kb.dma(kb.sp, [(p_[:, 1:Ls + 1], A.pT[OFF_HY + j * 128:OFF_HY + (j + 1) * 128, off:off + Ls])], dst=p_)
            ts(kb, kb.dve, u_[:, :], p_[:, 1:Ls + 1], P.pd[:, 12 + j:13 + j], P.pd[:, 36 + j:37 + j], ALU.mult, ALU.add,
               reads=[p_, P.pd], writes=[u_])
            stt(kb, kb.dve, u_[:, :], p_[:, 0:Ls], P.pd[:, j:j + 1], u_[:, :], ALU.mult, ALU.add, reads=[p_, P.pd, u_], writes=[u_])
            stt(kb, kb.dve, u_[:, :], p_[:, 2:Ls + 2], P.pd[:, 24 + j:25 + j], u_[:, :], ALU.mult, ALU.add, reads=[p_, P.pd, u_], writes=[u_])
            kb.dma(kb.sp, [(A.uTd[j * 128:(j + 1) * 128, off:off + Ls], u_[:, :])], src=u_)
        kb.barrier()
    with ExitStack() as st:
        h1T = kb.sb(st, "h1T", [65, Ls], F32)
        h2T = kb.sb(st, "h2T", [65, Ls], F32)
        zT = kb.sb(st, "zT", [33, Ls], F32)
        w1 = kb.sb(st, "w1", [33, 64], F32)
        w2 = kb.sb(st, "w2", [64, 64], F32)
        w3 = kb.sb(st, "w3", [65, 2048], F32)
        sc = kb.sb(st, "sc", [64, 4], F32)
        negpi = kb.sb(st, "negpi", [128, 1], F32)
        hb = kb.sb(st, "hb", [1, 2, 512], F32)
        alt = kb.sb(st, "alt", [128, 1], BF16)
        altr = kb.sb(st, "altr", [1, Ls], BF16)
        tmpa = kb.sb(st, "tmpa", [64, 512], F32)
        tmpb = kb.sb(st, "tmpb", [64, 512], F32)
        tmpi = kb.sb(st, "tmpi", [64, 512], mybir.dt.int32)
        kb.dma(kb.sp, [(zT[:, :], getattr(C.d, "hz_" + tag))], dst=zT)
        kb.dma(kb.sp, [(w1[:, :], A.hy_w1[li])], dst=w1)
        kb.dma(kb.sp, [(w2[:, :], A.hy_w2[li])], dst=w2)
        kb.dma(kb.sp, [(w3[0:64, :], A.hy_w3[li]), (w3[64:65, :], A.hy_b3[li])], dst=w3)
        kb.dma(kb.sp, [(sc[:, 0:1], A.hy_b1[li]), (sc[:, 1:2], A.hy_freq1[li]), (sc[:, 2:3], A.hy_b2[li]), (sc[:, 3:4], A.hy_freq2[li])], dst=sc)
        kb.dma(kb.sp, [(hb[:, :, :], A.hy_bias[li:li + 1])], dst=hb)
        kb.dma(kb.sp, [(alt[:, :], getattr(C.d, "alt_" + tag)[0:128, :])], dst=alt)
        kb.dma(kb.sp, [(altr[:, :], getattr(C.d, "altr_" + tag))], dst=altr)
        memset(kb, kb.dve, negpi[:, :], -PI, writes=[negpi])
        memset(kb, kb.dve, h2T[64:65, :], 1.0, writes=[h2T])

        def sin_layer(dst, wt, kdim, src, bcol, fcol):
            for (b0, n) in blks:
                ps = kb.next_ps()
                mm(kb, ps[0:64, 0:n], ps, [(wt[0:kdim, :], src[0:kdim, b0:b0 + n])], reads=[wt, src])
                ts(kb, kb.dve, tmpa[:, 0:n], ps[0:64, 0:n], sc[:, bcol:bcol + 1], sc[:, fcol:fcol + 1], ALU.add, ALU.mult,
                   reads=[ps, sc], writes=[tmpa])
                ts(kb, kb.dve, tmpa[:, 0:n], tmpa[:, 0:n], 1.0 / (2 * PI), None, ALU.mult, None, reads=[tmpa], writes=[tmpa])
                cp(kb, kb.dve, tmpi[:, 0:n], tmpa[:, 0:n], reads=[tmpa], writes=[tmpi])
                cp(kb, kb.dve, tmpb[:, 0:n], tmpi[:, 0:n], reads=[tmpi], writes=[tmpb])
                tt(kb, kb.dve, tmpa[:, 0:n], tmpa[:, 0:n], tmpb[:, 0:n], ALU.subtract, reads=[tmpa, tmpb], writes=[tmpa])
                ts(kb, kb.dve, tmpb[:, 0:n], tmpa[:, 0:n], 0.5, None, ALU.is_gt, None, reads=[tmpa], writes=[tmpb])
                tt(kb, kb.dve, tmpa[:, 0:n], tmpa[:, 0:n], tmpb[:, 0:n], ALU.subtract, reads=[tmpa, tmpb], writes=[tmpa])
                ts(kb, kb.dve, tmpb[:, 0:n], tmpa[:, 0:n], -0.5, None, ALU.is_lt, None, reads=[tmpa], writes=[tmpb])
                tt(kb, kb.dve, tmpa[:, 0:n], tmpa[:, 0:n], tmpb[:, 0:n], ALU.add, reads=[tmpa, tmpb], writes=[tmpa])
                act(kb, dst[0:64, b0:b0 + n], tmpa[:, 0:n], AF.Sin, reads=[tmpa], writes=[dst], scale=2 * PI)

        sin_layer(h1T, w1, 33, zT, 0, 1)
        sin_layer(h2T, w2, 64, h1T, 2, 3)

        hk = kb.sb(st, "hk", [128, nT, 2, 512], BF16)
        utm = kb.sb(st, "utm", [128, nT, 512], BF16)
        yre = kb.sb(st, "yre", [128, nT, 512], BF16)
        yim = kb.sb(st, "yim", [128, nT, 512], BF16)
        yny = kb.sb(st, "yny", [1, 512], BF16)
        nyf = kb.sb(st, "nyf", [1, 2, 512], F32)
        zTt = kb.sb(st, "zTt", [128, 4, Ls], BF16)
        for o in range(2):
            with ExitStack() as s2:
                wins = [kb.sb(s2, f"win{i}", [128, 512], F32) for i in range(2)]
                hf = [kb.sb(s2, f"hf{i}", [128, 512], F32) for i in range(2)]
                wsrc = getattr(C.d, "hwin_" + tag)
                for pt in range(nT):
                    win = wins[pt % 2]
                    kb.dma(kb.sp, [(win[:, :], wsrc[pt * 128:(pt + 1) * 128, :])], dst=win)
                    for d in range(2):
                        ps = kb.next_ps()
                        cb = (o * 2 + d) * 512
                        mm(kb, ps[:, :], ps, [(h2T[0:65, pt * 128:(pt + 1) * 128], w3[0:65, cb:cb + 512])], reads=[h2T, w3])
                        tt(kb, kb.dve, hf[d][:, :], ps[:, :], win[:, :], ALU.mult, reads=[ps, win], writes=[hf[d]])
                    if pt == 0:
                        tt(kb, kb.dve, hf[0][0:1, :], hf[0][0:1, :], hb[0:1, o, :], ALU.add, reads=[hf[0], hb], writes=[hf[0]])
                        memset(kb, kb.dve, hf[1][0:1, :], 0.0, writes=[hf[1]])
                    tt(kb, kb.dve, hk[:, pt, 0, :], hf[0][:, :], hf[1][:, :], ALU.add, reads=hf, writes=[hk])
                    tt(kb, kb.dve, hk[:, pt, 1, :], hf[1][:, :], hf[0][:, :], ALU.subtract, reads=hf, writes=[hk])
                kb.barrier()
            with ExitStack() as s2:
                srcs = [kb.sb(s2, f"src{i}", [128, Ls], BF16) for i in range(2)]
                for cc in range(4):
                    sr = srcs[cc % 2]
                    if o == 0:
                        kb.dma(kb.sp, [(sr[:, :], A.uTd[cc * 128:(cc + 1) * 128, off:off + Ls])], dst=sr)
                        rd = sr
                        rap = sr
                    else:
                        rd = zTt
                    for g in range(0, nT, 8):
                        ph = kb.psb[(g // 8) % 2]
                        ng = min(8, nT - g)
                        for k8 in range(ng):
                            pt = g + k8
                            src_ap = sr[:, pt * 128:(pt + 1) * 128] if o == 0 else zTt[:, cc, pt * 128:(pt + 1) * 128]
                            tr(kb, ph[:, k8 * 128:(k8 + 1) * 128], ph, src_ap, C.identb[:, :], reads=[rd, C.identb])
                        for k8 in range(ng):
                            pt = g + k8
                            cp(kb, kb.act if k8 % 2 else kb.dve, utm[:, pt, cc * 128:(cc + 1) * 128], ph[:, k8 * 128:(k8 + 1) * 128],
                               reads=[ph], writes=[utm])
                kb.barrier()
            with ExitStack() as s2:
                ring = [kb.sb(s2, f"cs{i}", [128, nT, 2, 128], BF16) for i in range(2)]
                psb_ = [kb.sb(s2, f"pq{i}", [128, 512], F32) for i in range(2)]
                t_ = [kb.sb(s2, f"tq{i}", [128, 512], F32) for i in range(2)]
                cv = dC.rearrange("(sc p) f -> p sc f", p=128)
                sv = dS.rearrange("(sc p) f -> p sc f", p=128)
                loads = [(lambda t, fc=fc: [(t[:, :, 0, :], cv[:, :, fc * 128:(fc + 1) * 128]), (t[:, :, 1, :], sv[:, :, fc * 128:(fc + 1) * 128])])
                         for fc in range(nT)]

                def compf(fc, slot):
                    pA, pB, pP, pQ = kb.next_ps(), kb.next_ps(), kb.next_ps(), kb.next_ps()
                    mm(kb, pP[:, :], pP, [(slot[:, s_, 0, :], hk[:, s_, 0, :]) for s_ in range(nT)], reads=[slot, hk])
                    mm(kb, pQ[:, :], pQ, [(slot[:, s_, 1, :], hk[:, s_, 1, :]) for s_ in range(nT)], reads=[slot, hk])
                    mm(kb, pA[:, :], pA, [(slot[:, s_, 0, :], utm[:, s_, :]) for s_ in range(nT)], reads=[slot, utm])
                    mm(kb, pB[:, :], pB, [(slot[:, s_, 1, :], utm[:, s_, :]) for s_ in range(nT)], reads=[slot, utm])
                    cp(kb, kb.act, psb_[0][:, :], pP[:, :], reads=[pP], writes=[psb_[0]])
                    cp(kb, kb.act, psb_[1][:, :], pQ[:, :], reads=[pQ], writes=[psb_[1]])
                    tt(kb, kb.dve, t_[0][:, :], pA[:, :], psb_[0][:, :], ALU.mult, reads=[pA, psb_[0]], writes=[t_[0]])
                    tt(kb, kb.dve, t_[1][:, :], pB[:, :], psb_[1][:, :], ALU.mult, reads=[pB, psb_[1]], writes=[t_[1]])
                    tt(kb, kb.dve, yre[:, fc, :], t_[0][:, :], t_[1][:, :], ALU.add, reads=t_, writes=[yre])
                    tt(kb, kb.dve, t_[0][:, :], pB[:, :], psb_[0][:, :], ALU.mult, reads=[pB, psb_[0]], writes=[t_[0]])
                    tt(kb, kb.dve, t_[1][:, :], pA[:, :], psb_[1][:, :], ALU.mult, reads=[pA, psb_[1]], writes=[t_[1]])
                    tt(kb, kb.dve, yim[:, fc, :], t_[0][:, :], t_[1][:, :], ALU.subtract, reads=t_, writes=[yim])

                stream(kb, ring, loads, compf, kb.sp)
                pn = kb.next_ps()
                mm(kb, pn[0:1, :], pn, [(alt[:, 0:1], utm[:, s_, :]) for s_ in range(nT)], reads=[alt, utm])
                cp(kb, kb.dve, nyf[:, 0, :], pn[0:1, :], reads=[pn], writes=[nyf])
                pn2 = kb.next_ps()
                mm(kb, pn2[0:1, :], pn2, [(alt[:, 0:1], hk[:, s_, 0, :]) for s_ in range(nT)], reads=[alt, hk])
                tt(kb, kb.dve, yny[:, :], pn2[0:1, :], nyf[:, 0, :], ALU.mult, reads=[pn2, nyf], writes=[yny])
                ts(kb, kb.dve, yre[0:1, 0, :], yre[0:1, 0, :], 0.5, None, ALU.mult, None, reads=[yre], writes=[yre])
                kb.barrier()
            with ExitStack() as s2:
                ring = [kb.sb(s2, f"ci{i}", [128, nT, 2, 256], BF16) for i in range(2)]
                gts = [kb.sb(s2, f"gt{i}", [128, 256], BF16) for i in range(2)]
                ots = [kb.sb(s2, f"ot{i}", [128, 256], BF16) for i in range(2)]
                r32 = kb.sb(s2, "r32", [128, 256], F32)
                cv = dC.rearrange("(fc p) t -> p fc t", p=128)
                sv = dS.rearrange("(fc p) t -> p fc t", p=128)
                loads = [(lambda t, b0=b0, n=n: [(t[:, :, 0, 0:n], cv[:, :, b0:b0 + n]), (t[:, :, 1, 0:n], sv[:, :, b0:b0 + n])])
                         for (b0, n) in iblk]
                gc = [0]

                def compi(j, slot):
                    b0, n = iblk[j]
                    for cc in range(4):
                        ps = kb.next_ps()
                        terms = [(yre[:, fc, cc * 128:(cc + 1) * 128], slot[:, fc, 0, 0:n]) for fc in range(nT)]
                        terms += [(yim[:, fc, cc * 128:(cc + 1) * 128], slot[:, fc, 1, 0:n]) for fc in range(nT)]
                        terms += [(yny[0:1, cc * 128:(cc + 1) * 128], altr[0:1, b0:b0 + n])]
                        mm(kb, ps[:, 0:n], ps, terms, reads=[yre, yim, yny, altr, slot])
                        gt = gts[gc[0] % 2]
                        ot = ots[gc[0] % 2]
                        gc[0] += 1
                        grow_ = (4 if o == 0 else 8) + cc
                        kb.dma(kb.sp, [(gt[:, 0:n], A.uTd[grow_ * 128:(grow_ + 1) * 128, off + b0:off + b0 + n])], dst=gt)
                        if o == 0:
                            stt(kb, kb.dve, zTt[:, cc, b0:b0 + n], ps[:, 0:n], inv_scale, gt[:, 0:n], ALU.mult, ALU.mult,
                                reads=[ps, gt], writes=[zTt])
                        else:
                            stt(kb, kb.dve, ot[:, 0:n], ps[:, 0:n], inv_scale, gt[:, 0:n], ALU.mult, ALU.mult, reads=[ps, gt], writes=[ot])
                            kb.dma(kb.sp, [(A.olT[1536 + cc * 128:1536 + (cc + 1) * 128, off + b0:off + b0 + n], ot[:, 0:n])], src=ot)

                stream(kb, ring, loads, compi, kb.sp)
                kb.barrier()
        kb.barrier()
```
